# Optimizing a Trainium2 kernel written in Bass

```python
import math
import jax, jax.numpy as jnp
from jax import lax
import numpy as np

D_MODEL = 1024
BATCH = 8
SEQ = 4096
DEPTH = 1

CONV_DIM = D_MODEL
CONV_WIDTH = 31
GMLP_DIM = D_MODEL
GMLP_HEADS = 8
GMLP_HEAD_DIM = GMLP_DIM // GMLP_HEADS
CHUNK = 128
N_EXPERTS = 32
TOP_K = 4
D_EXPERT = D_MODEL
SWIGLU_ALPHA = 1.702
SWIGLU_LIMIT = 7.0
MOE_BLOCK = 256
LN_EPS = 1e-5
DEEPNORM_ALPHA = (2.0 * DEPTH) ** 0.25
DEEPNORM_BETA = (8.0 * DEPTH) ** -0.25
N_MOD = 6
IN_WIDTHS = (CONV_DIM, CONV_DIM, GMLP_DIM, GMLP_DIM, D_MODEL, D_MODEL)
IN_DIM = sum(IN_WIDTHS)
IN_SPLITS = tuple(int(s) for s in np.cumsum(IN_WIDTHS)[:-1])

kernel_name = "conformer_gmlp_moe_deepnorm_block"


def _layer_norm(x):
    xf = x.astype(jnp.float32)
    mu = jnp.mean(xf, axis=-1, keepdims=True)
    var = jnp.mean(jnp.square(xf - mu), axis=-1, keepdims=True)
    return ((xf - mu) * lax.rsqrt(var + LN_EPS)).astype(x.dtype)


def _ln_affine(x, g, b):
    return _layer_norm(x) * g + b


def _conformer_conv(a_val, a_gate, conv_w, conv_b, ln_g, ln_b, w_pa, b_pa):
    z = a_val * jax.nn.sigmoid(a_gate)
    z = jnp.pad(z, ((0, 0), (CONV_WIDTH - 1, 0), (0, 0)))
    z = lax.conv_general_dilated(
        z, conv_w[:, None, :], window_strides=(1,), padding="VALID",
        dimension_numbers=("NWC", "WIO", "NWC"),
        feature_group_count=CONV_DIM) + conv_b
    z = jax.nn.silu(_ln_affine(z, ln_g, ln_b))
    return z @ w_pa + b_pa


def _chunked_spatial_gating(u, v, ln_g, ln_b, w_s, b_s, w_pb, b_pb):
    bsz, seq, _ = u.shape
    n_chunks = seq // CHUNK
    u = jax.nn.gelu(u, approximate=False)
    v = _ln_affine(jax.nn.gelu(v, approximate=False), ln_g, ln_b)
    v = v.reshape(bsz, n_chunks, CHUNK, GMLP_HEADS, GMLP_HEAD_DIM)
    causal = jnp.tril(jnp.ones((CHUNK, CHUNK), dtype=bool))
    w_causal = jnp.where(causal[None], w_s, 0.0)
    mixed = jnp.einsum("hts,bnshd->bnthd", w_causal, v) + b_s.T[:, :, None]
    gated = u * mixed.reshape(bsz, seq, GMLP_DIM)
    return gated @ w_pb + b_pb


def _mixer(h, w_in, b_in, conv_w, conv_b, ln_a_g, ln_a_b, w_pa, b_pa,
           ln_v_g, ln_v_b, w_s, b_s, w_pb, b_pb, w_out, b_out):
    proj = h @ w_in + b_in
    a_val, a_gate, u, v, g_a, g_b = jnp.split(proj, IN_SPLITS, axis=-1)
    y_a = _conformer_conv(a_val, a_gate, conv_w, conv_b, ln_a_g, ln_a_b, w_pa, b_pa)
    y_b = _chunked_spatial_gating(u, v, ln_v_g, ln_v_b, w_s, b_s, w_pb, b_pb)
    merged = jax.nn.sigmoid(g_a) * y_a + jax.nn.sigmoid(g_b) * y_b
    return merged @ w_out + b_out


def _moe(h, w_router, b_router, w_up, b_up, w_down, b_down):
    bsz, seq, dm = h.shape
    n_tok = bsz * seq
    hf = h.reshape(n_tok, dm)
    logits = (hf @ w_router + b_router).astype(jnp.float32)
    top_logit, top_idx = lax.top_k(logits, TOP_K)
    top_w = jax.nn.softmax(top_logit, axis=-1).astype(h.dtype)
    n_assign = n_tok * TOP_K
    flat_e = top_idx.reshape(n_assign)
    flat_tok = jnp.repeat(jnp.arange(n_tok, dtype=jnp.int32), TOP_K)
    flat_w = top_w.reshape(n_assign)
    order = jnp.argsort(flat_e)
    sorted_e = flat_e[order]
    counts = jnp.bincount(flat_e, length=N_EXPERTS)
    padded = (counts + MOE_BLOCK - 1) // MOE_BLOCK * MOE_BLOCK
    start = jnp.cumsum(counts) - counts
    padded_end = jnp.cumsum(padded)
    padded_start = padded_end - padded
    dest = padded_start[sorted_e] + jnp.arange(n_assign, dtype=jnp.int32) - start[sorted_e]
    n_blocks = -(-n_assign // MOE_BLOCK) + N_EXPERTS
    n_rows = n_blocks * MOE_BLOCK
    row_tok = jnp.zeros((n_rows,), jnp.int32).at[dest].set(flat_tok[order])
    row_w = jnp.zeros((n_rows,), h.dtype).at[dest].set(flat_w[order])
    block_expert = jnp.minimum(
        jnp.searchsorted(padded_end, jnp.arange(n_blocks) * MOE_BLOCK, side="right"),
        N_EXPERTS - 1)
    xg = hf[row_tok].reshape(n_blocks, MOE_BLOCK, dm)

    def expert_block(args):
        xb, e = args
        z = xb @ w_up[e] + b_up[e]
        glu, lin = jnp.split(z, 2, axis=-1)
        glu = jnp.minimum(glu, SWIGLU_LIMIT)
        lin = jnp.clip(lin, -SWIGLU_LIMIT, SWIGLU_LIMIT)
        act = glu * jax.nn.sigmoid(SWIGLU_ALPHA * glu) * (lin + 1.0)
        return act @ w_down[e] + b_down[e]

    yg = lax.map(expert_block, (xg, block_expert)).reshape(n_rows, dm)
    out = jnp.zeros_like(hf).at[row_tok].add(yg * row_w[:, None])
    return out.reshape(bsz, seq, dm)


def setup_inputs(seed: int = 0) -> dict:
    key = jax.random.key(seed)
    ks = jax.random.split(key, 32)
    L = DEPTH

    def nrm(k, shape, scale):
        return jax.random.normal(k, shape, jnp.float32) * scale

    def gain(k, shape):
        return 1.0 + nrm(k, shape, 0.02)

    return {
        "x": nrm(ks[0], (BATCH, SEQ, D_MODEL), 1.0),
        "c": nrm(ks[1], (BATCH, D_MODEL), 1.0),
        "w_ada": nrm(ks[2], (L, D_MODEL, N_MOD * D_MODEL), 0.5 * D_MODEL ** -0.5),
        "b_ada": nrm(ks[3], (L, N_MOD * D_MODEL), 0.02),
        "w_in": nrm(ks[4], (L, D_MODEL, IN_DIM), D_MODEL ** -0.5),
        "b_in": nrm(ks[5], (L, IN_DIM), 0.02),
        "conv_w": nrm(ks[6], (L, CONV_WIDTH, CONV_DIM), CONV_WIDTH ** -0.5),
        "conv_b": nrm(ks[7], (L, CONV_DIM), 0.02),
        "ln_a_g": gain(ks[8], (L, CONV_DIM)),
        "ln_a_b": nrm(ks[9], (L, CONV_DIM), 0.02),
        "w_pa": nrm(ks[10], (L, CONV_DIM, D_MODEL), DEEPNORM_BETA * CONV_DIM ** -0.5),
        "b_pa": nrm(ks[11], (L, D_MODEL), 0.02),
        "ln_v_g": gain(ks[12], (L, GMLP_DIM)),
        "ln_v_b": nrm(ks[13], (L, GMLP_DIM), 0.02),
        "w_s": nrm(ks[14], (L, GMLP_HEADS, CHUNK, CHUNK), CHUNK ** -0.5),
        "b_s": gain(ks[15], (L, GMLP_HEADS, CHUNK)),
        "w_pb": nrm(ks[16], (L, GMLP_DIM, D_MODEL), DEEPNORM_BETA * GMLP_DIM ** -0.5),
        "b_pb": nrm(ks[17], (L, D_MODEL), 0.02),
        "w_out": nrm(ks[18], (L, D_MODEL, D_MODEL), DEEPNORM_BETA * D_MODEL ** -0.5),
        "b_out": nrm(ks[19], (L, D_MODEL), 0.02),
        "post1_g": gain(ks[20], (L, D_MODEL)),
        "post1_b": nrm(ks[21], (L, D_MODEL), 0.02),
        "w_router": nrm(ks[22], (L, D_MODEL, N_EXPERTS), D_MODEL ** -0.5),
        "b_router": nrm(ks[23], (L, N_EXPERTS), 0.01),
        "w_up": nrm(ks[24], (L, N_EXPERTS, D_MODEL, 2 * D_EXPERT), D_MODEL ** -0.5),
        "b_up": nrm(ks[25], (L, N_EXPERTS, 2 * D_EXPERT), 0.02),
        "w_down": nrm(ks[26], (L, N_EXPERTS, D_EXPERT, D_MODEL), DEEPNORM_BETA * D_EXPERT ** -0.5),
        "b_down": nrm(ks[27], (L, N_EXPERTS, D_MODEL), 0.02),
        "post2_g": gain(ks[28], (L, D_MODEL)),
        "post2_b": nrm(ks[29], (L, D_MODEL), 0.02),
    }


def reference(x, c, w_ada, b_ada, w_in, b_in, conv_w, conv_b, ln_a_g, ln_a_b, w_pa, b_pa,
              ln_v_g, ln_v_b, w_s, b_s, w_pb, b_pb, w_out, b_out, post1_g, post1_b,
              w_router, b_router, w_up, b_up, w_down, b_down, post2_g, post2_b):
    cond = jax.nn.silu(c)
    for l in range(DEPTH):
        mod = cond @ w_ada[l] + b_ada[l]
        shift1, scale1, gate1, shift2, scale2, gate2 = [
            m[:, None, :] for m in jnp.split(mod, N_MOD, axis=-1)]
        h = _layer_norm(x) * (1.0 + scale1) + shift1
        y = _mixer(h, w_in[l], b_in[l], conv_w[l], conv_b[l], ln_a_g[l], ln_a_b[l],
                   w_pa[l], b_pa[l], ln_v_g[l], ln_v_b[l], w_s[l], b_s[l],
                   w_pb[l], b_pb[l], w_out[l], b_out[l])
        x = _ln_affine(DEEPNORM_ALPHA * x + gate1 * y, post1_g[l], post1_b[l])
        h = _layer_norm(x) * (1.0 + scale2) + shift2
        y = _moe(h, w_router[l], b_router[l], w_up[l], b_up[l], w_down[l], b_down[l])
        x = _ln_affine(DEEPNORM_ALPHA * x + gate2 * y, post2_g[l], post2_b[l])
    return x
```

```python
import contextlib
import numpy as np
import concourse.bass as bass
import concourse.mybir as mybir
from concourse.bass_utils import run_bass_kernel_spmd
from concourse.alu_op_type import AluOpType as ALU

F32 = mybir.dt.float32
BF16 = mybir.dt.bfloat16
I32 = mybir.dt.int32
AF = mybir.ActivationFunctionType
AX = mybir.AxisListType

T = 4096
D = 1024
NT = T // 128
NB = T // 512
E = 32
KW = 31
MB = 512
NMB = 64
ALPHA = 2.0 ** 0.25
EPS = 1e-5
SW_ALPHA = 1.702
SW_LIMIT = 7.0
ENGS = ("tensor", "vector", "scalar", "gpsimd", "sync")


class Prog:
    def __init__(self, nc):
        self.nc = nc
        self.ops = {e: [] for e in ENGS}
        self.sem_val = {}
        self.waited = {e: {} for e in ENGS}
        self.last_write = {}
        self.readers = {}
        self.bank = 0

    def _need(self, eng, tok, waits):
        if tok is None:
            return
        sem, val, teng = tok
        if teng == "tensor" and eng == "tensor":
            return
        if self.waited[eng].get(sem, 0) >= val:
            return
        if waits.get(sem, 0) < val:
            waits[sem] = val

    def op(self, eng, fn, reads=(), writes=(), dma_sem=None, n_inc=1):
        ps_reads = [r for r in reads if isinstance(r, tuple) and r and r[0] == "ps"]
        if ps_reads:
            reads = [r for r in reads if r not in ps_reads]
            writes = list(writes) + ps_reads
        waits = {}
        for r in reads:
            self._need(eng, self.last_write.get(r), waits)
        for w in writes:
            self._need(eng, self.last_write.get(w), waits)
            for t in self.readers.get(w, {}).values():
                self._need(eng, t, waits)
        for s, v in waits.items():
            self.waited[eng][s] = v
        if dma_sem is not None:
            sem, amt, teng = "D_" + dma_sem, 16, "dma"
        else:
            sem, amt, teng = "E_" + eng, 1, eng
        val = self.sem_val.get(sem, 0) + amt * n_inc
        self.sem_val[sem] = val
        tok = (sem, val, teng)
        for r in reads:
            self.readers.setdefault(r, {})[sem] = tok
        for w in writes:
            self.last_write[w] = tok
            self.readers[w] = {}
        self.ops[eng].append((fn, list(waits.items()), (sem, amt)))
        return tok

    def barrier(self):
        for eng in ENGS:
            waits = []
            for s, v in self.sem_val.items():
                if self.waited[eng].get(s, 0) < v:
                    waits.append((s, v))
                    self.waited[eng][s] = v
            self.ops[eng].append((None, waits, None))
        self.last_write = {}
        self.readers = {}

    def alias(self, new_keys, old_keys):
        toks = {}
        for k in old_keys:
            t = self.last_write.get(k)
            if t is not None:
                if toks.get(t[0], (None, 0))[1] < t[1]:
                    toks[t[0]] = t
            for t in self.readers.get(k, {}).values():
                if toks.get(t[0], (None, 0))[1] < t[1]:
                    toks[t[0]] = t
        for k in new_keys:
            self.last_write[k] = None
            self.readers[k] = dict(toks)

    def next_bank(self):
        b = self.bank
        self.bank = (b + 1) % 8
        return b

    def emit(self):
        nc = self.nc
        names = sorted(self.sem_val.keys())
        with contextlib.ExitStack() as st:
            sems = {n: st.enter_context(nc.semaphore(n)) for n in names}
            block = st.enter_context(nc.Block())

            def mk(engname):
                def body(e):
                    for fn, waits, inc in self.ops[engname]:
                        for s, v in waits:
                            e.wait_ge(sems[s], v)
                        if fn is None:
                            continue
                        ins = fn(e)
                        for i_ in (ins if isinstance(ins, list) else [ins]):
                            i_.then_inc(sems[inc[0]], inc[1])
                return body

            block.tensor(mk("tensor"))
            block.vector(mk("vector"))
            block.scalar(mk("scalar"))
            block.gpsimd(mk("gpsimd"))
            block.sync(mk("sync"))


class Arena:
    def __init__(self, nc, base, limit):
        self.nc, self.ptr, self.limit, self.n = nc, base, limit, 0

    def at(self, off, name, shape, dt):
        self.n += 1
        return self.nc.alloc_sbuf_tensor_at(f"{name}_{self.n}", list(shape), dt, offset=off)

    def alloc(self, name, shape, dt, nbytes=None):
        esz = 2 if dt == BF16 else 4
        if nbytes is None:
            nbytes = int(np.prod(shape[1:])) * esz
        nbytes = (nbytes + 31) // 32 * 32
        off = self.ptr
        self.ptr += nbytes
        assert self.ptr <= self.limit, (name, self.ptr, self.limit)
        return self.at(off, name, shape, dt), off


def build_program(stage=3, stop=None, moe_blocks=None):
    try:
        return _build_program(stage, stop, moe_blocks)
    except _StopBuild as s:
        return s.nc


class _StopBuild(Exception):
    def __init__(self, nc):
        self.nc = nc


def _build_program(stage, stop, moe_blocks):
    nc = bass.Bass("TRN2", target_bir_lowering=False)
    P = Prog(nc)

    def din(name, shape, dt=F32):
        return nc.dram_tensor(name, list(shape), dt, kind="ExternalInput").ap()

    x_d = din("x", [T, D])
    c_d = din("c", [1, D])
    w_ada = din("w_ada", [D, 6 * D]); b_ada = din("b_ada", [1, 6 * D])
    w_in = din("w_in", [D, 6 * D]); b_in = din("b_in", [6, D])
    conv_w = din("conv_w", [KW, D]); conv_b = din("conv_b", [1, D])
    ln_a_g = din("ln_a_g", [1, D]); ln_a_b = din("ln_a_b", [1, D])
    w_pa = din("w_pa", [D, D]); b_pa = din("b_pa", [1, D])
    ln_v_g = din("ln_v_g", [1, D]); ln_v_b = din("ln_v_b", [1, D])
    w_s = din("w_s", [8, 128, 128]); b_s = din("b_s", [1, D])
    w_pb = din("w_pb", [D, D]); b_pb = din("b_pb", [1, D])
    w_out = din("w_out", [D, D]); b_out = din("b_out", [1, D])
    post1_g = din("post1_g", [1, D]); post1_b = din("post1_b", [1, D])
    w_router = din("w_router", [D, E]); b_router = din("b_router", [1, E])
    if stage >= 2:
        w_up = din("w_up", [E * D, 2 * D]); b_up = din("b_up", [E, 2 * D])
        w_down = din("w_down", [E * D, D]); b_down = din("b_down", [E, D])
        post2_g = din("post2_g", [1, D]); post2_b = din("post2_b", [1, D])

    okind = "ExternalOutput"
    out_d = nc.dram_tensor("out", [T, D], F32, kind=okind).ap()
    dbg = stage < 3
    x1_scr = nc.dram_tensor("x1_scr", [T, D], F32, kind=okind if dbg else "Internal").ap()
    xn2_scr = nc.dram_tensor("xn2_scr", [T, D], BF16, kind="Internal").ap()
    wbf = nc.dram_tensor("wbf", [9, 128, 8 * D], BF16, kind="Internal").ap()
    lg_scr = nc.dram_tensor("lg_scr", [128, NT * E], F32, kind=okind if dbg else "Internal").ap()
    xg_scr = nc.dram_tensor("xg_scr", [NMB * MB, D], BF16, kind="Internal").ap()
    yg_scr = nc.dram_tensor("yg_scr", [NMB * MB, D], F32, kind="Internal").ap()

    KB = 1024
    LIMIT = 229376
    ar = Arena(nc, 16640, LIMIT)
    identb, _ = ar.alloc("identb", [128, 128], BF16)
    identf, _ = ar.alloc("identf", [128, 128], F32)
    onesb, _ = ar.alloc("onesb", [128, 128], BF16)
    onesf, _ = ar.alloc("onesf", [128, 128], F32)
    VT, _ = ar.alloc("VT", [128, 8, 50], F32)
    cond, _ = ar.alloc("cond", [128, 8], F32)
    WcT, _ = ar.alloc("WcT", [128, 8, 128], BF16)
    bs_hl, _ = ar.alloc("bs_hl", [33, D], BF16)
    bv_hl, _ = ar.alloc("bv_hl", [33, D], BF16)
    bo_hl, _ = ar.alloc("bo_hl", [33, D], BF16)
    wr, _ = ar.alloc("wr", [128, 8, E], F32)
    br_row, _ = ar.alloc("br_row", [1, E], F32)
    mhalf, _ = ar.alloc("mhalf", [128, 2], F32)
    ztile, _ = ar.alloc("ztile", [128, D], BF16)
    epsc, _ = ar.alloc("epsc", [128, 2], F32)
    BC, _ = ar.alloc("BC", [128, 6, D], F32)
    BC_P1G, BC_P1B, BC_LVG, BC_LVB, BC_G1, BC_G2 = range(6)
    STS = []
    for i in range(4):
        s6, _ = ar.alloc(f"st6_{i}", [128, 4, 2, 6], F32)
        m2, _ = ar.alloc(f"mv_{i}", [128, 4, 2], F32)
        r1, _ = ar.alloc(f"rstd_{i}", [128, 4], F32)
        n1, _ = ar.alloc(f"nb_{i}", [128, 4], F32)
        STS.append((s6, m2, r1, n1))
    base = ar.ptr
    WT_off = [base + i * 16 * KB for i in range(6)]
    X_off = base + 96 * KB
    X_size = LIMIT - X_off

    ps = [nc.alloc_psum_tensor(f"ps{i}", [128, 512], F32) for i in range(8)]

    def psf(i):
        return ps[i]

    def psb(i):
        return ps[i][:].bitcast(BF16)

    K = lambda *a: tuple(a)

    dbg_d = nc.dram_tensor("dbg", [128, 8192], F32, kind="ExternalOutput").ap() if stop is not None else None

    def checkpoint(k, dumps=()):
        if stop != k:
            return
        i = 0
        for (ap, c0) in dumps:
            n = ap.shape[-1]
            for s0 in range(0, n, 1024):
                s1 = min(n, s0 + 1024)
                P.op("gpsimd", lambda e, ap=ap, c0=c0, s0=s0, s1=s1: e.dma_start(
                    out=dbg_d[0:ap.shape[0], c0 + s0:c0 + s1], in_=ap[:, s0:s1]),
                    reads=list(P.last_write.keys()), writes=[K("dbg", i)], dma_sem=f"dbg{i}")
                i += 1
        P.barrier()
        P.emit()
        raise _StopBuild(nc)

    def mm_group(bk, pairs, reads, out_ap=None):
        out_ap = psf(bk)[:] if out_ap is None else out_ap

        def fn(e):
            ins = None
            n = len(pairs)
            for i, (l, r) in enumerate(pairs):
                ins = e.matmul(out_ap, lhsT=l, rhs=r, start=(i == 0), stop=(i == n - 1))
            return ins
        P.op("tensor", fn, reads=reads, writes=[K("ps", bk)])

    su = Arena(nc, base, LIMIT)
    V, _ = su.alloc("V", [50, D], F32)
    V2, _ = su.alloc("V2", [6, D], F32)
    io_i, _ = su.alloc("io_i", [128, 128], I32)
    condrep, _ = su.alloc("condrep", [128, 8, 128], F32)
    badarow, _ = su.alloc("badarow", [1, 6 * D], F32)
    modbc, _ = su.alloc("modbc", [128, 6 * D], F32)
    wa = [su.alloc(f"wa{i}", [128, 8, 512], F32)[0] for i in range(2)]
    wsld, _ = su.alloc("wsld", [128, 8, 128], F32)
    wsT, _ = su.alloc("wsT", [128, 8, 128], F32)
    brow, _ = su.alloc("brow", [33, D], F32)
    brow2, _ = su.alloc("brow2", [33, D], F32)
    bhi, _ = su.alloc("bhi", [33, D], BF16)

    P.op("gpsimd", lambda e: e.iota(io_i[:], pattern=[[1, 128]], base=0, channel_multiplier=-1), writes=["io_i"])
    P.op("vector", lambda e: e.tensor_scalar(out=identb[:], in0=io_i[:], scalar1=0, scalar2=None, op0=ALU.is_equal),
         reads=["io_i"], writes=["identb"])
    P.op("vector", lambda e: e.tensor_scalar(out=identf[:], in0=io_i[:], scalar1=0, scalar2=None, op0=ALU.is_equal),
         reads=["io_i"], writes=["identf"])
    P.op("gpsimd", lambda e: e.memset(onesb[:], 1.0), writes=["onesb"])
    P.op("gpsimd", lambda e: e.memset(onesf[:], 1.0), writes=["onesf"])
    P.op("gpsimd", lambda e: e.memset(mhalf[:], -0.5), writes=["mhalf"])
    P.op("gpsimd", lambda e: e.memset(epsc[:], EPS), writes=["epsc"])
    P.op("gpsimd", lambda e: e.memset(bs_hl[:], 0.0), writes=["bs_hl"])
    P.op("gpsimd", lambda e: e.memset(bv_hl[:], 0.0), writes=["bv_hl"])
    P.op("gpsimd", lambda e: e.memset(bo_hl[:], 0.0), writes=["bo_hl"])
    P.op("gpsimd", lambda e: e.memset(V[:], 0.0), writes=["Vz"])

    vrows = [(b_in, 0, 6), (conv_b, 6, 1), (ln_a_g, 7, 1), (ln_a_b, 8, 1), (b_pa, 9, 1), (b_pb, 10, 1),
             (c_d, 11, 1), (conv_w, 12, KW)]
    for i, (src, r0, n) in enumerate(vrows):
        P.op("sync", lambda e, src=src, r0=r0, n=n: e.dma_start(out=V[r0:r0 + n, :], in_=src),
             reads=["Vz"], writes=[K("V", i)], dma_sem=f"V{i}")
    vkeys = [K("V", i) for i in range(len(vrows))]

    def vt_transposes(src, nrows, col0, rkeys, wkey):
        for half in range(2):
            bk = P.next_bank()

            def tr(e, half=half, bk=bk):
                ins = None
                for q in range(4):
                    ch = half * 4 + q
                    ins = e.transpose(out=psf(bk)[:, q * 64:q * 64 + nrows], in_=src[0:nrows, ch * 128:(ch + 1) * 128],
                                      identity=identf[0:nrows, 0:nrows])
                return ins
            P.op("tensor", tr, reads=list(rkeys) + ["identf"], writes=[K("ps", bk)])
            P.op("vector", lambda e, half=half, bk=bk: e.tensor_copy(
                out=VT[:, half * 4:half * 4 + 4, col0:col0 + nrows],
                in_=psf(bk)[:, 0:256].rearrange("p (q r) -> p q r", q=4)[:, :, 0:nrows]),
                reads=[K("ps", bk)], writes=[K(wkey, half)])

    vt_transposes(V, 44, 0, vkeys, "VT")
    VTK = [K("VT", 0), K("VT", 1)]
    cv = [su.alloc(f"cv{i}", [128, 8, D], BF16)[0] for i in range(2)]
    w_in_v0 = w_in.rearrange("(ko p) n -> p ko n", p=128)
    chunks = [
        [(0, 512, w_in_v0[:, :, 0:512]), (512, 512, w_in_v0[:, :, D:D + 512])],
        [(0, 512, w_in_v0[:, :, 512:D]), (512, 512, w_in_v0[:, :, D + 512:2 * D])],
        [(0, D, w_in_v0[:, :, 2 * D:3 * D])], [(0, D, w_in_v0[:, :, 3 * D:4 * D])],
        [(0, D, w_in_v0[:, :, 4 * D:5 * D])], [(0, D, w_in_v0[:, :, 5 * D:6 * D])],
        [(0, D, w_pa.rearrange("(ko p) n -> p ko n", p=128))], [(0, D, w_pb.rearrange("(ko p) n -> p ko n", p=128))],
        [(0, D, w_out.rearrange("(ko p) n -> p ko n", p=128))],
    ]
    def convert_chunk(ci):
        parts = chunks[ci]
        s = ci % 2
        if ci < 8:
            dst = wbf[ci].rearrange("p (k n) -> p k n", k=8)
            for pi, (c0, n, srcap) in enumerate(parts):
                P.op("gpsimd", lambda e, dst=dst, c0=c0, n=n, srcap=srcap: e.dma_start(out=dst[:, :, c0:c0 + n], in_=srcap),
                     writes=[K("wbf", ci, pi)], dma_sem="wbfd")
            return
        for pi, (c0, n, srcap) in enumerate(parts):
            P.op("gpsimd", lambda e, s=s, c0=c0, n=n, srcap=srcap: e.dma_start(out=cv[s][:, :, c0:c0 + n], in_=srcap),
                 writes=[K("cv", s, pi)], dma_sem=f"cv{s}_{pi}")
        ck_ = [K("cv", s, pi) for pi in range(len(parts))]
        for h in range(2):
            P.op("vector", lambda e, s=s, h=h: e.tensor_tensor(
                out=cv[s][:, :, h * 512:(h + 1) * 512], in0=cv[s][:, :, h * 512:(h + 1) * 512],
                in1=BC[:, BC_G1, h * 512:(h + 1) * 512].unsqueeze(1).to_broadcast([128, 8, 512]), op=ALU.mult),
                reads=ck_ + [K("BC", BC_G1)], writes=ck_)
        P.op("gpsimd", lambda e, s=s, ci=ci: e.dma_start(out=wbf[ci], in_=cv[s][:].rearrange("p k n -> p (k n)")),
             reads=ck_, writes=[K("wbf", ci)], dma_sem=f"wbf{s}")

    for ci in range(8):
        convert_chunk(ci)
    P.op("scalar", lambda e: e.activation(out=cond[:], in_=VT[:, :, 11], func=AF.Silu), reads=VTK, writes=["cond"])
    for ko in range(8):
        P.op("vector", lambda e, ko=ko: e.tensor_scalar(out=condrep[:, ko, :], in0=onesf[:], scalar1=cond[:, ko:ko + 1],
                                                        scalar2=None, op0=ALU.mult),
             reads=["cond", "onesf"], writes=[K("condrep", ko)])
    P.op("sync", lambda e: e.dma_start(out=badarow[:], in_=b_ada), writes=["badarow"], dma_sem="badarow")
    w_ada_v = w_ada.rearrange("(ko p) n -> p ko n", p=128)
    for cc in range(12):
        s = cc % 2
        P.op("sync", lambda e, cc=cc, s=s: e.dma_start(out=wa[s][:], in_=w_ada_v[:, :, cc * 512:(cc + 1) * 512]),
             writes=[K("wa", s)], dma_sem=f"wa{s}")
        bk = P.next_bank()
        mm_group(bk, [(condrep[:, ko, :], wa[s][:, ko, :]) for ko in range(8)]
                 + [(onesf[0:1, :], badarow[0:1, cc * 512:(cc + 1) * 512])],
                 [K("wa", s), "badarow", "onesf"] + [K("condrep", ko) for ko in range(8)])
        P.op("scalar", lambda e, cc=cc, bk=bk: e.copy(out=modbc[:, cc * 512:(cc + 1) * 512], in_=psf(bk)[:]),
             reads=[K("ps", bk)], writes=[K("modbc", cc)])
    mkeys = [K("modbc", cc) for cc in range(12)]
    P.op("vector", lambda e: e.tensor_copy(out=BC[:, BC_G1, :], in_=modbc[:, 2 * D:3 * D]), reads=mkeys, writes=[K("BC", BC_G1)])
    P.op("vector", lambda e: e.tensor_copy(out=BC[:, BC_G2, :], in_=modbc[:, 5 * D:6 * D]), reads=mkeys, writes=[K("BC", BC_G2)])
    for i in range(6):
        P.op("sync", lambda e, i=i: e.dma_start(out=V2[i:i + 1, :], in_=modbc[i:i + 1, i * D:(i + 1) * D]),
             reads=mkeys, writes=[K("V2", i)], dma_sem=f"V2_{i}")
    vt_transposes(V2, 6, 44, [K("V2", i) for i in range(6)], "VTm")
    VTK = VTK + [K("VTm", 0), K("VTm", 1)]
    C_SH1, C_SC1, C_SH2, C_SC2 = 44, 45, 47, 48
    for col in (C_SC1, C_SC2):
        P.op("vector", lambda e, col=col: e.tensor_scalar(out=VT[:, :, col], in0=VT[:, :, col], scalar1=1.0, scalar2=None, op0=ALU.add),
             reads=VTK, writes=VTK)
    for slot, src in ((BC_P1G, post1_g), (BC_P1B, post1_b), (BC_LVG, ln_v_g), (BC_LVB, ln_v_b)):
        P.op("sync", lambda e, slot=slot, src=src: e.dma_start(out=BC[:, slot, :], in_=src.to_broadcast([128, D])),
             writes=[K("BC", slot)], dma_sem=f"BC{slot}")
    P.op("sync", lambda e: e.dma_start(out=wsld[:], in_=w_s.rearrange("h t s -> t h s")), writes=["wsld"], dma_sem="wsld")
    for half in range(2):
        bk = P.next_bank()

        def tr(e, half=half, bk=bk):
            ins = None
            for q in range(4):
                ins = e.transpose(out=psf(bk)[:, q * 128:(q + 1) * 128], in_=wsld[:, half * 4 + q, :], identity=identf[:])
            return ins
        P.op("tensor", tr, reads=["wsld", "identf"], writes=[K("ps", bk)])
        P.op("vector", lambda e, half=half, bk=bk: e.tensor_copy(
            out=wsT[:, half * 4:half * 4 + 4, :], in_=psf(bk)[:].rearrange("p (q t) -> p q t", q=4)),
            reads=[K("ps", bk)], writes=[K("wsT", half)])
    P.op("gpsimd", lambda e: e.affine_select(out=WcT[:], in_=wsT[:], pattern=[[0, 8], [1, 128]], compare_op=ALU.is_ge,
                                             fill=0.0, base=0, channel_multiplier=-1),
         reads=[K("wsT", 0), K("wsT", 1)], writes=["WcT"])

    def hilo(dst, dkey, prep):
        for row in (0, 32):
            prep(row)
        P.op("vector", lambda e: e.tensor_copy(out=dst[0:1, :], in_=brow[0:1, :]), reads=["brow0"], writes=[dkey])
        P.op("vector", lambda e: e.tensor_copy(out=bhi[32:33, :], in_=brow[32:33, :]), reads=["brow32"], writes=["bhi"])
        P.op("vector", lambda e: e.tensor_tensor(out=dst[32:33, :], in0=brow[32:33, :], in1=bhi[32:33, :], op=ALU.subtract),
             reads=["brow32", "bhi"], writes=[dkey])

    def load_row(src):
        def prep(row):
            P.op("sync", lambda e: e.dma_start(out=brow[row:row + 1, :], in_=src), writes=[f"brow{row}"], dma_sem=f"brow{row}")
        return prep
    hilo(bs_hl, "bs_hl", load_row(b_s))
    hilo(bv_hl, "bv_hl", load_row(b_in[3:4, :]))

    def prep_bo(row):
        P.op("sync", lambda e: e.dma_start(out=brow2[row:row + 1, :], in_=b_out), writes=[f"brow2_{row}"], dma_sem=f"brow2_{row}")
        P.op("vector", lambda e: e.tensor_tensor(out=brow[row:row + 1, :], in0=brow2[row:row + 1, :], in1=BC[row:row + 1, BC_G1, :],
                                                 op=ALU.mult),
             reads=[f"brow2_{row}", K("BC", BC_G1)], writes=[f"brow{row}"])
    hilo(bo_hl, "bo_hl", prep_bo)
    convert_chunk(8)
    P.op("sync", lambda e: e.dma_start(out=wr[:], in_=w_router.rearrange("(ko p) e -> p ko e", p=128)), writes=["wr"], dma_sem="wr")
    P.op("sync", lambda e: e.dma_start(out=br_row[:], in_=b_router), writes=["br_row"], dma_sem="br_row")
    checkpoint("setup", [(VT[:].rearrange("p a b -> p (a b)"), 0), (BC[:, BC_G1, :], 512), (BC[:, BC_P1G, :], 1536),
                         (WcT[:].rearrange("p a b -> p (a b)"), 2560), (bs_hl[:], 3584), (bo_hl[:], 4608), (cond[:], 5632)])

    P.barrier()

    WS = [ar.at(WT_off[i], f"WS{i}", [128, 8, D], BF16) for i in range(2)]
    A_off = [WT_off[2] + i * 8 * KB for i in range(8)] + [X_off + 16 * KB]
    Afm = [ar.at(o, "Afm", [128, 8, 512], BF16) for o in A_off]
    Atm = [ar.at(o, "Atm", [128, 4, D], BF16) for o in A_off]
    F1 = ar.at(X_off, "F1", [128, 4, D], F32)
    mx = Arena(nc, X_off + 24 * KB, LIMIT)
    zA, _ = mx.alloc("zA", [128, 8, 544], BF16)
    DG = [mx.alloc(f"DG{i}", [128, KW, 128], BF16)[0] for i in range(2)]
    sgt = [mx.alloc(f"sgt{i}", [128, 512], BF16)[0] for i in range(2)]
    tf = [mx.alloc(f"tf{i}", [128, 512], F32)[0] for i in range(2)]
    tfn = [mx.alloc(f"tfn{i}", [128, 512], F32)[0] for i in range(2)]
    xn2f = [mx.alloc(f"xn2f{i}", [128, D], F32)[0] for i in range(2)]
    h2T1, _ = mx.alloc("h2T", [128, 8, 128], F32)
    h2T = [h2T1, h2T1]
    lgs = [mx.alloc(f"lgs{i}", [128, E], F32)[0] for i in range(2)]

    A_XN = A_GATED = 0
    A_HT, A_GU, A_V = 1, 2, 3
    A_ZC = A_TA = 4
    A_SQ = A_XN2 = 5
    A_ZS, A_SGA, A_SGB = 6, 7, 8
    xn_t, hT, gu, vtok = Atm[A_XN], Afm[A_HT], Afm[A_GU], Atm[A_V]
    zc, sq, zs, gated = Afm[A_ZC], Afm[A_SQ], Afm[A_ZS], Afm[A_GATED]
    sgA, sgB, tA, xn2b = Afm[A_SGA], Afm[A_SGB], Afm[A_TA], Atm[A_XN2]
    merged = tA
    xnk = [K("xn", a) for a in range(4)]
    gatedk = [K("gated", a, hh) for a in range(4) for hh in range(2)]
    zck = [K("zc", fc) for fc in range(8)]
    tAk = [K("tA", fc) for fc in range(8)]
    sqk = [K("sq", fc) for fc in range(8)]
    xn2bk = [K("xn2b", a) for a in range(4)]

    x_v = x_d.rearrange("(b a p) d -> b p a d", a=4, p=128)
    x1_v = x1_scr.rearrange("(b a p) d -> b p a d", a=4, p=128)
    xn2_v = xn2_scr.rearrange("(b a p) d -> b p a d", a=4, p=128)
    w_in_v = w_in.rearrange("(ko p) n -> p ko n", p=128)
    w_pa_v = w_pa.rearrange("(ko p) n -> p ko n", p=128)
    w_pb_v = w_pb.rearrange("(ko p) n -> p ko n", p=128)
    w_out_v = w_out.rearrange("(ko p) n -> p ko n", p=128)

    P.op("gpsimd", lambda e: e.memset(zA[:], 0.0), writes=[K("zA", fc) for fc in range(8)])
    if stage >= 2:
        P.op("gpsimd", lambda e: e.memset(ztile[:], 0.0), writes=["ztile"])
        xg_z = xg_scr.rearrange("(p c r) d -> c p r d", p=128, c=16)

    wcount = [0]

    def load_w(ci):
        s = wcount[0] % 2
        wcount[0] += 1
        P.op("sync", lambda e, s=s, ci=ci: e.dma_start(out=WS[s][:].rearrange("p k n -> p (k n)"), in_=wbf[ci]),
             writes=[K("W", s, 0), K("W", s, 1)], dma_sem=f"W{s}")
        return s

    def wkeys(s):
        return [K("W", s, 0), K("W", s, 1)]

    def ln_stats4(src_fn, sset, rkeys_fn, tag, tiles=(0, 1, 2, 3), use_act=False):
        st, mvt, rs, nbt = STS[sset]
        for a in tiles:
            for h in range(2):
                P.op("vector", lambda e, a=a, h=h: e.bn_stats(out=st[:, a, h, :], in_=src_fn(a)[:, h * 512:(h + 1) * 512]),
                     reads=rkeys_fn(a), writes=[K(tag + "st", a, h)])
            P.op("vector", lambda e, a=a: e.bn_aggr(out=mvt[:, a, :], in_=st[:, a, :, :].rearrange("p h s -> p (h s)")),
                 reads=[K(tag + "st", a, 0), K(tag + "st", a, 1)], writes=[K(tag + "mv", a)])
        a0, a1 = tiles[0], tiles[-1] + 1
        mvk = [K(tag + "mv", a) for a in tiles]
        if use_act:
            P.op("scalar", lambda e: e.activation(out=rs[:, a0:a1], in_=mvt[:, a0:a1, 1], func=AF.Sqrt, bias=epsc[:, 0:1], scale=1.0),
                 reads=mvk, writes=[tag + "rs"])
            P.op("vector", lambda e: e.reciprocal(out=rs[:, a0:a1], in_=rs[:, a0:a1]), reads=[tag + "rs"], writes=[tag + "rs"])
        else:
            P.op("vector", lambda e: e.tensor_scalar(out=rs[:, a0:a1], in0=mvt[:, a0:a1, 1], scalar1=EPS, scalar2=None, op0=ALU.add),
                 reads=mvk, writes=[tag + "rs"])
            P.op("gpsimd", lambda e: e.tensor_tensor(out=rs[:, a0:a1], in0=rs[:, a0:a1], in1=mhalf[:, 0:1].to_broadcast([128, a1 - a0]),
                                                     op=ALU.pow), reads=[tag + "rs"], writes=[tag + "rs"])
        P.op("vector", lambda e: e.scalar_tensor_tensor(out=nbt[:, a0:a1], in0=mvt[:, a0:a1, 0], scalar=-1.0, in1=rs[:, a0:a1],
                                                        op0=ALU.mult, op1=ALU.mult),
             reads=mvk + [tag + "rs"], writes=[tag + "nb"])
        return [tag + "rs", tag + "nb"], rs, nbt

    for tb in range(NB):
        P.op("sync", lambda e, tb=tb: e.dma_start(out=F1[:], in_=x_v[tb]), writes=[K("F1", a) for a in range(4)], dma_sem="F1")
        P.alias(xnk, gatedk)
        if tb == 0:
            checkpoint("x", [(F1[:, 0, :], 0), (F1[:, 3, :], 1024)])
        sk, rs, nbt = ln_stats4(lambda a: F1[:, a, :], 0, lambda a: [K("F1", a)], "l1")
        for a in range(4):
            P.op("scalar", lambda e, a=a, rs=rs, nbt=nbt: e.activation(out=xn_t[:, a, :], in_=F1[:, a, :], func=AF.Identity,
                                                                       bias=nbt[:, a:a + 1], scale=rs[:, a:a + 1]),
                 reads=[K("F1", a)] + sk, writes=[K("xn", a)])
        if tb == 0:
            checkpoint("ln1", [(xn_t[:, 0, :], 0), (xn_t[:, 3, :], 1024)])
        for kp in range(4):
            bk = P.next_bank()

            def tr(e, kp=kp, bk=bk):
                ins = None
                for q in range(2):
                    ko = kp * 2 + q
                    for a in range(4):
                        ins = e.transpose(out=psb(bk)[:, q * 512 + a * 128:q * 512 + (a + 1) * 128],
                                          in_=xn_t[:, a, ko * 128:(ko + 1) * 128], identity=identb[:])
                return ins
            P.op("tensor", tr, reads=xnk + ["identb"], writes=[K("ps", bk)])
            for q in range(2):
                ko = kp * 2 + q
                if kp % 2 == 0:
                    P.op("vector", lambda e, ko=ko, q=q, bk=bk: e.tensor_scalar(
                        out=hT[:, ko, :], in0=psb(bk)[:, q * 512:(q + 1) * 512], scalar1=VT[:, ko, C_SC1:C_SC1 + 1],
                        scalar2=VT[:, ko, C_SH1:C_SH1 + 1], op0=ALU.mult, op1=ALU.add),
                        reads=[K("ps", bk)] + VTK, writes=[K("hT", ko)])
                else:
                    P.op("scalar", lambda e, ko=ko, q=q, bk=bk: e.activation(
                        out=hT[:, ko, :], in_=psb(bk)[:, q * 512:(q + 1) * 512], func=AF.Identity,
                        bias=VT[:, ko, C_SH1:C_SH1 + 1], scale=VT[:, ko, C_SC1:C_SC1 + 1]),
                        reads=[K("ps", bk)] + VTK, writes=[K("hT", ko)])
        hTk = [K("hT", ko) for ko in range(8)]
        if tb == 0:
            checkpoint("xn", [(xn_t[:, 0, :], 0), (xn_t[:, 3, :], 1024)])
            checkpoint("hT", [(hT[:].rearrange("p a b -> p (a b)"), 0)])

        for t2 in range(2):
            s = load_w(t2)
            for c4 in range(4):
                fc = t2 * 4 + c4
                bv, bg = P.next_bank(), P.next_bank()
                mm_group(bv, [(WS[s][:, ko, c4 * 128:(c4 + 1) * 128], hT[:, ko, :]) for ko in range(8)], wkeys(s) + hTk)
                mm_group(bg, [(WS[s][:, ko, 512 + c4 * 128:512 + (c4 + 1) * 128], hT[:, ko, :]) for ko in range(8)], wkeys(s) + hTk)
                sg = sgt[fc % 2]
                P.op("scalar", lambda e, fc=fc, bg=bg, sg=sg: e.activation(out=sg[:], in_=psf(bg)[:], func=AF.Sigmoid,
                                                                         bias=VT[:, fc, 1:2], scale=1.0),
                     reads=[K("ps", bg)] + VTK, writes=[K("sgt", fc % 2)])
                P.op("vector", lambda e, fc=fc, bv=bv, sg=sg: e.scalar_tensor_tensor(
                    out=zA[:, fc, 30:542], in0=psf(bv)[:], scalar=VT[:, fc, 0:1], in1=sg[:], op0=ALU.add, op1=ALU.mult),
                    reads=[K("ps", bv), K("sgt", fc % 2)] + VTK, writes=[K("zA", fc)])
        if tb == 0:
            checkpoint("glu", [(zA[:, fc, 30:542], fc * 512) for fc in range(8)])
        if stage >= 2:
            for c in (2 * tb, 2 * tb + 1):
                P.op("gpsimd", lambda e, c=c: e.dma_start(out=xg_z[c], in_=ztile[:].unsqueeze(1).to_broadcast([128, 16, D])),
                     reads=["ztile"], writes=[K("xgz", c)], dma_sem="xgz")
        s = load_w(2)
        for fc in range(8):
            bk = P.next_bank()
            mm_group(bk, [(WS[s][:, ko, fc * 128:(fc + 1) * 128], hT[:, ko, :]) for ko in range(8)], wkeys(s) + hTk)
            P.op("scalar", lambda e, fc=fc, bk=bk: e.activation(out=gu[:, fc, :], in_=psf(bk)[:], func=AF.Gelu,
                                                                bias=VT[:, fc, 2:3], scale=1.0),
                 reads=[K("ps", bk)] + VTK, writes=[K("gu", fc)])
        if tb == 0:
            checkpoint("gu", [(gu[:].rearrange("p a b -> p (a b)"), 0)])
        P.alias(zck, tAk)
        P.alias(sqk, xn2bk)
        for fc in range(8):
            dg = DG[fc % 2]
            P.op("vector", lambda e, fc=fc, dg=dg: e.tensor_tensor(
                out=dg[:], in0=identb[:].unsqueeze(1).to_broadcast([128, KW, 128]),
                in1=VT[:, fc, 12:12 + KW].unsqueeze(2).to_broadcast([128, KW, 128]), op=ALU.mult),
                reads=["identb"] + VTK, writes=[K("DG", fc % 2)])
            bk = P.next_bank()
            mm_group(bk, [(dg[:, j, :], zA[:, fc, j:j + 512]) for j in range(KW)], [K("DG", fc % 2), K("zA", fc)])
            P.op("scalar", lambda e, fc=fc, bk=bk: e.activation(out=zc[:, fc, :], in_=psf(bk)[:], func=AF.Identity,
                                                                bias=VT[:, fc, 6:7], scale=1.0),
                 reads=[K("ps", bk)] + VTK, writes=[K("zc", fc)])
            P.op("scalar", lambda e, fc=fc, bk=bk: e.activation(out=sq[:, fc, :], in_=psf(bk)[:], func=AF.Square,
                                                                bias=VT[:, fc, 6:7], scale=1.0),
                 reads=[K("ps", bk)] + VTK, writes=[K("sq", fc)])
        zAk = [K("zA", fc) for fc in range(8)]
        P.op("gpsimd", lambda e: e.tensor_copy(out=zA[:, :, 0:30], in_=zA[:, :, 512:542]), reads=zAk, writes=zAk)
        if tb == 0:
            checkpoint("conv", [(zc[:].rearrange("p a b -> p (a b)"), 0), (sq[:].rearrange("p a b -> p (a b)"), 4096)])
        b1, b2 = P.next_bank(), P.next_bank()
        mm_group(b1, [(onesb[:], zc[:, fc, :]) for fc in range(8)], zck + ["onesb"])
        mm_group(b2, [(onesb[:], sq[:, fc, :]) for fc in range(8)], sqk + ["onesb"])
        mean_t, var_t = tf[0], tf[1]
        rs_t, nmr_t = var_t, mean_t
        P.op("vector", lambda e, b1=b1: e.tensor_scalar(out=mean_t[:], in0=psf(b1)[:], scalar1=1.0 / D, scalar2=None, op0=ALU.mult),
             reads=[K("ps", b1)], writes=["mean_t"])
        P.op("vector", lambda e: e.tensor_tensor(out=var_t[:], in0=mean_t[:], in1=mean_t[:], op=ALU.mult),
             reads=["mean_t"], writes=["var_t"])
        P.op("vector", lambda e, b2=b2: e.scalar_tensor_tensor(out=var_t[:], in0=psf(b2)[:], scalar=1.0 / D, in1=var_t[:],
                                                               op0=ALU.mult, op1=ALU.subtract),
             reads=[K("ps", b2), "var_t"], writes=["var_t"])
        P.op("scalar", lambda e: e.activation(out=var_t[:], in_=var_t[:], func=AF.Sqrt, bias=epsc[:, 0:1], scale=1.0),
             reads=["var_t", "epsc"], writes=["var_t"])
        P.op("vector", lambda e: e.reciprocal(out=rs_t[:], in_=var_t[:]), reads=["var_t"], writes=["var_t"])
        P.op("vector", lambda e: e.scalar_tensor_tensor(out=nmr_t[:], in0=mean_t[:], scalar=-1.0, in1=rs_t[:], op0=ALU.mult, op1=ALU.mult),
             reads=["mean_t", "var_t"], writes=["mean_t"])
        for fc in range(8):
            tn = tfn[fc % 2]
            tnk = K("tfn", fc % 2)
            P.op("vector", lambda e, fc=fc, tn=tn: e.tensor_tensor(out=tn[:], in0=zc[:, fc, :], in1=rs_t[:], op=ALU.mult),
                 reads=[K("zc", fc), "var_t"], writes=[tnk])
            P.op("gpsimd", lambda e, fc=fc, tn=tn: e.tensor_tensor(out=tn[:], in0=tn[:], in1=nmr_t[:], op=ALU.add),
                 reads=[tnk, "mean_t"], writes=[tnk])
            P.op("scalar", lambda e, fc=fc, tn=tn: e.activation(out=zs[:, fc, :], in_=tn[:], func=AF.Silu,
                                                               bias=VT[:, fc, 8:9], scale=VT[:, fc, 7:8]),
                 reads=[tnk] + VTK, writes=[K("zs", fc)])
        s = load_w(3)
        for a in range(4):
            for nh in range(2):
                bk = P.next_bank()
                mm_group(bk, [(hT[:, ko, a * 128:(a + 1) * 128], WS[s][:, ko, nh * 512:(nh + 1) * 512]) for ko in range(8)]
                         + [(onesb[0:33, :], bv_hl[0:33, nh * 512:(nh + 1) * 512])], wkeys(s) + hTk + ["bv_hl", "onesb"])
                P.op("scalar", lambda e, a=a, nh=nh, bk=bk: e.activation(out=vtok[:, a, nh * 512:(nh + 1) * 512], in_=psf(bk)[:],
                                                                       func=AF.Gelu),
                     reads=[K("ps", bk)], writes=[K("v", a, nh)])
        sk, rs, nbt = ln_stats4(lambda a: vtok[:, a, :], 1, lambda a: [K("v", a, 0), K("v", a, 1)], "lv")
        for a in range(4):
            vk = [K("v", a, 0), K("v", a, 1)]
            P.op("scalar", lambda e, a=a, rs=rs, nbt=nbt: e.activation(out=vtok[:, a, :], in_=vtok[:, a, :], func=AF.Identity,
                                                                       bias=nbt[:, a:a + 1], scale=rs[:, a:a + 1]),
                 reads=vk + sk, writes=vk)
            P.op("vector", lambda e, a=a: e.tensor_tensor(out=vtok[:, a, :], in0=vtok[:, a, :], in1=BC[:, BC_LVG, :], op=ALU.mult),
                 reads=vk + [K("BC", BC_LVG)], writes=vk)
            P.op("gpsimd", lambda e, a=a: e.tensor_tensor(out=vtok[:, a, :], in0=vtok[:, a, :], in1=BC[:, BC_LVB, :], op=ALU.add),
                 reads=vk + [K("BC", BC_LVB)], writes=vk)
        if tb == 0:
            checkpoint("v", [(vtok[:].rearrange("p a b -> p (a b)"), 0)])
        for gi, dst, name in ((4, sgA, "sgA"), (5, sgB, "sgB")):
            s = load_w(gi)
            for fc in range(8):
                bk = P.next_bank()
                mm_group(bk, [(WS[s][:, ko, fc * 128:(fc + 1) * 128], hT[:, ko, :]) for ko in range(8)], wkeys(s) + hTk)
                P.op("scalar", lambda e, fc=fc, bk=bk, dst=dst, gi=gi: e.activation(out=dst[:, fc, :], in_=psf(bk)[:], func=AF.Sigmoid,
                                                                                  bias=VT[:, fc, gi:gi + 1], scale=1.0),
                     reads=[K("ps", bk)] + VTK, writes=[K(name, fc)])
        if tb == 0:
            checkpoint("gates", [(sgA[:].rearrange("p a b -> p (a b)"), 0), (sgB[:].rearrange("p a b -> p (a b)"), 4096)])
        if tb == 0:
            checkpoint("lna", [(zs[:].rearrange("p a b -> p (a b)"), 0), (rs_t[:], 4096), (mean_t[:], 4608)])
        P.alias(gatedk, xnk)
        for a in range(4):
            vk = [K("v", a, 0), K("v", a, 1)]
            for hh in range(2):
                bk = P.next_bank()

                def mix(e, a=a, hh=hh, bk=bk):
                    ins = None
                    for q in range(4):
                        h = hh * 4 + q
                        e.matmul(psf(bk)[:, q * 128:(q + 1) * 128], lhsT=vtok[:, a, h * 128:(h + 1) * 128], rhs=WcT[:, h, :],
                                 start=True, stop=False)
                        ins = e.matmul(psf(bk)[:, q * 128:(q + 1) * 128], lhsT=onesb[0:33, :], rhs=bs_hl[0:33, h * 128:(h + 1) * 128],
                                       start=False, stop=True)
                    return ins
                P.op("tensor", mix, reads=vk + ["WcT", "bs_hl", "onesb"], writes=[K("ps", bk)])
                P.op("vector", lambda e, a=a, hh=hh, bk=bk: e.tensor_tensor(
                    out=gated[:, hh * 4:hh * 4 + 4, a * 128:(a + 1) * 128], in0=gu[:, hh * 4:hh * 4 + 4, a * 128:(a + 1) * 128],
                    in1=psf(bk)[:].rearrange("p (q t) -> p q t", q=4), op=ALU.mult),
                    reads=[K("ps", bk)] + [K("gu", fc) for fc in range(hh * 4, hh * 4 + 4)], writes=[K("gated", a, hh)])
        if tb == 0:
            checkpoint("mix", [(gated[:].rearrange("p a b -> p (a b)"), 0)])
        P.alias(tAk, zck)
        s = load_w(6)
        for fc in range(8):
            bk = P.next_bank()
            mm_group(bk, [(WS[s][:, ko, fc * 128:(fc + 1) * 128], zs[:, ko, :]) for ko in range(8)],
                     wkeys(s) + [K("zs", ko) for ko in range(8)])
            P.op("vector", lambda e, fc=fc, bk=bk: e.scalar_tensor_tensor(
                out=tA[:, fc, :], in0=psf(bk)[:], scalar=VT[:, fc, 9:10], in1=sgA[:, fc, :], op0=ALU.add, op1=ALU.mult),
                reads=[K("ps", bk), K("sgA", fc)] + VTK, writes=[K("tA", fc)])
        s = load_w(7)
        for fc in range(8):
            bk = P.next_bank()
            mm_group(bk, [(WS[s][:, ko, fc * 128:(fc + 1) * 128], gated[:, ko, :]) for ko in range(8)], wkeys(s) + gatedk)
            P.op("vector", lambda e, fc=fc, bk=bk: e.scalar_tensor_tensor(
                out=sgB[:, fc, :], in0=psf(bk)[:], scalar=VT[:, fc, 10:11], in1=sgB[:, fc, :], op0=ALU.add, op1=ALU.mult),
                reads=[K("ps", bk), K("sgB", fc)] + VTK, writes=[K("sgB", fc)])
            P.op("gpsimd", lambda e, fc=fc: e.tensor_tensor(out=merged[:, fc, :], in0=tA[:, fc, :], in1=sgB[:, fc, :], op=ALU.add),
                 reads=[K("tA", fc), K("sgB", fc)], writes=[K("tA", fc)])
        if tb == 0:
            checkpoint("merge", [(merged[:].rearrange("p a b -> p (a b)"), 0)])
        s = load_w(8)
        P.alias(xn2bk, sqk)
        for a in range(4):
            for nh in range(2):
                bk = P.next_bank()
                mm_group(bk, [(merged[:, ko, a * 128:(a + 1) * 128], WS[s][:, ko, nh * 512:(nh + 1) * 512]) for ko in range(8)]
                         + [(onesb[0:33, :], bo_hl[0:33, nh * 512:(nh + 1) * 512])],
                         wkeys(s) + tAk + ["bo_hl", "onesb"])
                P.op("vector", lambda e, a=a, nh=nh, bk=bk: e.scalar_tensor_tensor(
                    out=F1[:, a, nh * 512:(nh + 1) * 512], in0=F1[:, a, nh * 512:(nh + 1) * 512], scalar=ALPHA, in1=psf(bk)[:],
                    op0=ALU.mult, op1=ALU.add), reads=[K("ps", bk), K("F1", a)], writes=[K("F1", a)])
        sk, rs, nbt = ln_stats4(lambda a: F1[:, a, :], 2, lambda a: [K("F1", a)], "p1")
        for a in range(4):
            fk = [K("F1", a)]
            P.op("scalar", lambda e, a=a, rs=rs, nbt=nbt: e.activation(out=F1[:, a, :], in_=F1[:, a, :], func=AF.Identity,
                                                                       bias=nbt[:, a:a + 1], scale=rs[:, a:a + 1]),
                 reads=fk + sk, writes=fk)
            P.op("vector", lambda e, a=a: e.tensor_tensor(out=F1[:, a, :], in0=F1[:, a, :], in1=BC[:, BC_P1G, :], op=ALU.mult),
                 reads=fk + [K("BC", BC_P1G)], writes=fk)
            P.op("gpsimd", lambda e, a=a: e.tensor_tensor(out=F1[:, a, :], in0=F1[:, a, :], in1=BC[:, BC_P1B, :], op=ALU.add),
                 reads=fk + [K("BC", BC_P1B)], writes=fk)
            P.op("sync", lambda e, a=a, tb=tb: e.dma_start(out=x1_v[tb][:, a, :], in_=F1[:, a, :]), reads=fk,
                 writes=[K("x1s", tb, a)], dma_sem=f"x1s{a}")
        sk, rs, nbt = ln_stats4(lambda a: F1[:, a, :], 3, lambda a: [K("F1", a)], "l2")
        for a in range(4):
            fk = [K("F1", a)]
            xf = xn2f[a % 2]
            xfk = [K("xn2f", a % 2)]
            P.op("scalar", lambda e, a=a, rs=rs, nbt=nbt, xf=xf: e.activation(out=xf[:], in_=F1[:, a, :], func=AF.Identity,
                                                                              bias=nbt[:, a:a + 1], scale=rs[:, a:a + 1]),
                 reads=fk + sk, writes=xfk)
            P.op("gpsimd", lambda e, a=a, xf=xf: e.tensor_copy(out=xn2b[:, a, :], in_=xf[:]), reads=xfk, writes=[K("xn2b", a)])
            bks = [P.next_bank(), P.next_bank()]
            for half in range(2):
                bk = bks[half]

                def tr(e, half=half, bk=bk, xf=xf):
                    ins = None
                    for q in range(4):
                        ko = half * 4 + q
                        ins = e.transpose(out=psf(bk)[:, q * 128:(q + 1) * 128], in_=xf[:, ko * 128:(ko + 1) * 128], identity=identf[:])
                    return ins
                P.op("tensor", tr, reads=xfk + ["identf"], writes=[K("ps", bk)])
                hT2 = h2T[a % 2]
                for q in range(4):
                    ko = half * 4 + q
                    if half == 0:
                        P.op("vector", lambda e, ko=ko, q=q, bk=bk, hT2=hT2: e.tensor_scalar(
                            out=hT2[:, ko, :], in0=psf(bk)[:, q * 128:(q + 1) * 128], scalar1=VT[:, ko, C_SC2:C_SC2 + 1],
                            scalar2=VT[:, ko, C_SH2:C_SH2 + 1], op0=ALU.mult, op1=ALU.add),
                            reads=[K("ps", bk)] + VTK, writes=[K("h2T", ko)])
                    else:
                        P.op("scalar", lambda e, ko=ko, q=q, bk=bk, hT2=hT2: e.activation(
                            out=hT2[:, ko, :], in_=psf(bk)[:, q * 128:(q + 1) * 128], func=AF.Identity,
                            bias=VT[:, ko, C_SH2:C_SH2 + 1], scale=VT[:, ko, C_SC2:C_SC2 + 1]),
                            reads=[K("ps", bk)] + VTK, writes=[K("h2T", ko)])
            bk = P.next_bank()
            hT2 = h2T[a % 2]
            mm_group(bk, [(hT2[:, ko, :], wr[:, ko, :]) for ko in range(8)] + [(onesf[0:1, :], br_row[0:1, :])],
                     [K("h2T", ko) for ko in range(8)] + ["wr", "br_row", "onesf"], out_ap=psf(bk)[:, 0:E])
            j = tb * 4 + a
            lg = lgs[j % 2]
            P.op("vector", lambda e, bk=bk, lg=lg: e.tensor_copy(out=lg[:], in_=psf(bk)[:, 0:E]),
                 reads=[K("ps", bk)], writes=[K("lgs", j % 2)])
            P.op("sync", lambda e, j=j, lg=lg: e.dma_start(out=lg_scr[:, j * E:(j + 1) * E], in_=lg[:]), reads=[K("lgs", j % 2)],
                 writes=[K("lg_scr", j)], dma_sem=f"lgs{j % 2}")
        P.op("sync", lambda e, tb=tb: e.dma_start(out=xn2_v[tb], in_=xn2b[:]), reads=xn2bk, writes=[K("xn2s", tb)], dma_sem="xn2s")
        if tb == 0:
            checkpoint("blk0", [(F1[:].rearrange("p a b -> p (a b)"), 0)])

    if stage == 1:
        P.barrier()
        P.emit()
        return nc

    P.barrier()

    IOA = bass.IndirectOffsetOnAxis
    xtop = Arena(nc, X_off, LIMIT)
    posI, _ = xtop.alloc("posI", [128, 4, NT], I32)
    gw, _ = xtop.alloc("gw", [128, NT, 4], F32)
    Gd, _ = xtop.alloc("Gd", [128, NT, E], F32)
    idxU, _ = xtop.alloc("idxU", [128, NMB, 8], I32)
    bupT, _ = xtop.alloc("bupT", [128, 16, NMB], F32)
    bdn_sb, _ = xtop.alloc("bdn_sb", [32, D], F32)
    moe_base = xtop.ptr
    rt = Arena(nc, WT_off[0], X_off)
    L, _ = rt.alloc("L", [128, NT, E], F32)
    m8, _ = rt.alloc("m8", [128, NT, 8], F32)
    d4, _ = rt.alloc("d4", [128, NT, 4], F32)
    es, _ = rt.alloc("es", [128, NT], F32)
    Mb, _ = rt.alloc("Mb", [128, NT * E], BF16)
    Ust, _ = rt.alloc("Ust", [128, 128], BF16)
    ioU, _ = rt.alloc("ioU", [128, 128], I32)
    within, _ = rt.alloc("within", [128, NT, E], F32)
    cnt, _ = rt.alloc("cnt", [128, NT, E], F32)
    cum, _ = rt.alloc("cum", [128, NT, E], F32)
    pos, _ = rt.alloc("pos", [128, NT, E], F32)
    sel, _ = rt.alloc("sel", [128, NT, E], F32)
    prod, _ = rt.alloc("prod", [128, NT, E], F32)
    posk, _ = rt.alloc("posk", [128, 4, NT], F32)
    tot, _ = rt.alloc("tot", [128, E], F32)
    nblk, _ = rt.alloc("nblk", [128, E], F32)
    pend, _ = rt.alloc("pend", [128, E], F32)
    pstart, _ = rt.alloc("pstart", [128, E], F32)
    iob_i, _ = rt.alloc("iob_i", [128, NMB], I32)
    iob, _ = rt.alloc("iob", [128, NMB], F32)
    cmpb, _ = rt.alloc("cmpb", [128, NMB, E], F32)
    blke, _ = rt.alloc("blke", [128, NMB], F32)
    pk_i, _ = rt.alloc("pk_i", [128, 8], I32)
    pk_f, _ = rt.alloc("pk_f", [128, 8], F32)
    idxf, _ = rt.alloc("idxf", [128, NMB, 8], F32)
    pid_i, _ = rt.alloc("pid_i", [128, 1], I32)
    pid_f, _ = rt.alloc("pid_f", [128, 1], F32)
    OH, _ = rt.alloc("OH", [32, NMB], F32)
    bup_sb, _ = rt.alloc("bup_sb", [32, 2 * D], F32)

    def V_(fn, reads, writes):
        P.op("vector", fn, reads=reads, writes=writes)

    for slot, src in ((BC_P1G, post2_g), (BC_P1B, post2_b)):
        P.op("sync", lambda e, slot=slot, src=src: e.dma_start(out=BC[:, slot, :], in_=src.to_broadcast([128, D])),
             writes=[K("BC", slot)], dma_sem=f"BC{slot}")
    P.op("sync", lambda e: e.dma_start(out=L[:].rearrange("p j e -> p (j e)"), in_=lg_scr), writes=["L"], dma_sem="L")
    P.op("sync", lambda e: e.dma_start(out=bup_sb[:], in_=b_up), writes=["bup_sb"], dma_sem="bup_sb")
    P.op("sync", lambda e: e.dma_start(out=bdn_sb[:], in_=b_down), writes=["bdn_sb"], dma_sem="bdn_sb")
    V_(lambda e: e.tensor_tensor(out=bdn_sb[:], in0=bdn_sb[:], in1=BC[0:32, BC_G2, :], op=ALU.mult), ["bdn_sb"], ["bdn_sb"])
    P.op("gpsimd", lambda e: e.iota(ioU[:], pattern=[[1, 128]], base=0, channel_multiplier=-1), writes=["ioU"])
    P.op("gpsimd", lambda e: e.iota(iob_i[:], pattern=[[1, NMB]], base=0, channel_multiplier=0), writes=["iob_i"])
    P.op("gpsimd", lambda e: e.iota(pk_i[:], pattern=[[128, 8]], base=0, channel_multiplier=1), writes=["pk_i"])
    P.op("gpsimd", lambda e: e.iota(pid_i[:], pattern=[[0, 1]], base=0, channel_multiplier=1), writes=["pid_i"])
    V_(lambda e: e.tensor_scalar(out=Ust[:], in0=ioU[:], scalar1=0, scalar2=None, op0=ALU.is_gt), ["ioU"], ["Ust"])
    V_(lambda e: e.tensor_copy(out=iob[:], in_=iob_i[:]), ["iob_i"], ["iob"])
    V_(lambda e: e.tensor_copy(out=pk_f[:], in_=pk_i[:]), ["pk_i"], ["pk_f"])
    V_(lambda e: e.tensor_copy(out=pid_f[:], in_=pid_i[:]), ["pid_i"], ["pid_f"])
    for j in range(NT):
        V_(lambda e, j=j: e.max(out=m8[:, j, :], in_=L[:, j, :]), ["L"], [K("m8", j)])
    m8k = [K("m8", j) for j in range(NT)]
    V_(lambda e: e.tensor_tensor(out=d4[:], in0=m8[:, :, 0:4], in1=m8[:, :, 0:1].to_broadcast([128, NT, 4]), op=ALU.subtract), m8k, ["d4"])
    P.op("scalar", lambda e: e.activation(out=d4[:], in_=d4[:], func=AF.Exp), reads=["d4"], writes=["d4"])
    V_(lambda e: e.tensor_reduce(out=es[:], in_=d4[:], axis=AX.X, op=ALU.add), ["d4"], ["es"])
    V_(lambda e: e.reciprocal(out=es[:], in_=es[:]), ["es"], ["es"])
    V_(lambda e: e.tensor_tensor(out=gw[:], in0=d4[:], in1=es[:].unsqueeze(2).to_broadcast([128, NT, 4]), op=ALU.mult), ["d4", "es"], ["gw"])
    V_(lambda e: e.tensor_tensor(out=Mb[:].rearrange("p (j e) -> p j e", e=E), in0=L[:], in1=m8[:, :, 3:4].to_broadcast([128, NT, E]),
                                 op=ALU.is_ge), ["L"] + m8k, ["Mb"])
    for half in range(2):
        bw, bc = P.next_bank(), P.next_bank()
        mm_group(bw, [(Ust[:], Mb[:, half * 512:(half + 1) * 512])], ["Ust", "Mb"])
        mm_group(bc, [(onesb[:], Mb[:, half * 512:(half + 1) * 512])], ["onesb", "Mb"])
        V_(lambda e, half=half, bw=bw: e.tensor_copy(out=within[:, half * 16:(half + 1) * 16, :],
                                                    in_=psf(bw)[:].rearrange("p (j e) -> p j e", e=E)), [K("ps", bw)], [K("within", half)])
        V_(lambda e, half=half, bc=bc: e.tensor_copy(out=cnt[:, half * 16:(half + 1) * 16, :],
                                                    in_=psf(bc)[:].rearrange("p (j e) -> p j e", e=E)), [K("ps", bc)], [K("cnt", half)])
    wk = [K("within", 0), K("within", 1)]
    ck = [K("cnt", 0), K("cnt", 1)]
    P.op("gpsimd", lambda e: e.memset(cum[:, 0, :], 0.0), writes=["cum"])
    for j in range(1, NT):
        V_(lambda e, j=j: e.tensor_tensor(out=cum[:, j, :], in0=cum[:, j - 1, :], in1=cnt[:, j - 1, :], op=ALU.add), ["cum"] + ck, ["cum"])
    V_(lambda e: e.tensor_tensor(out=tot[:], in0=cum[:, NT - 1, :], in1=cnt[:, NT - 1, :], op=ALU.add), ["cum"] + ck, ["tot"])
    P.op("gpsimd", lambda e: e.memset(nblk[:], 0.0), writes=["nblk"])
    for m in range(8):
        V_(lambda e, m=m: e.scalar_tensor_tensor(out=nblk[:], in0=tot[:], scalar=float(MB * m), in1=nblk[:], op0=ALU.is_gt, op1=ALU.add),
           ["tot", "nblk"], ["nblk"])
    V_(lambda e: e.tensor_copy(out=pend[:, 0:1], in_=nblk[:, 0:1]), ["nblk"], ["pend"])
    for ee in range(1, E):
        V_(lambda e, ee=ee: e.tensor_tensor(out=pend[:, ee:ee + 1], in0=pend[:, ee - 1:ee], in1=nblk[:, ee:ee + 1], op=ALU.add),
           ["pend", "nblk"], ["pend"])
    V_(lambda e: e.tensor_tensor(out=pstart[:], in0=pend[:], in1=nblk[:], op=ALU.subtract), ["pend", "nblk"], ["pstart"])
    V_(lambda e: e.scalar_tensor_tensor(out=pos[:], in0=pstart[:].unsqueeze(1).to_broadcast([128, NT, E]), scalar=float(MB), in1=cum[:],
                                        op0=ALU.mult, op1=ALU.add), ["pstart", "cum"], ["pos"])
    V_(lambda e: e.tensor_tensor(out=pos[:], in0=pos[:], in1=within[:], op=ALU.add), ["pos"] + wk, ["pos"])
    for k in range(4):
        V_(lambda e, k=k: e.tensor_tensor(out=sel[:], in0=L[:], in1=m8[:, :, k:k + 1].to_broadcast([128, NT, E]), op=ALU.is_equal),
           ["L"] + m8k, ["sel"])
        V_(lambda e: e.tensor_tensor(out=prod[:], in0=sel[:], in1=pos[:], op=ALU.mult), ["sel", "pos"], ["prod"])
        V_(lambda e, k=k: e.tensor_reduce(out=posk[:, k, :], in_=prod[:], axis=AX.X, op=ALU.add), ["prod"], [K("posk", k)])
        if k == 0:
            V_(lambda e, k=k: e.tensor_tensor(out=Gd[:], in0=sel[:], in1=gw[:, :, k:k + 1].to_broadcast([128, NT, E]), op=ALU.mult),
               ["sel", "gw"], ["Gd"])
        else:
            V_(lambda e, k=k: e.tensor_tensor(out=prod[:], in0=sel[:], in1=gw[:, :, k:k + 1].to_broadcast([128, NT, E]), op=ALU.mult),
               ["sel", "gw", "prod"], ["prod"])
            V_(lambda e: e.tensor_tensor(out=Gd[:], in0=Gd[:], in1=prod[:], op=ALU.add), ["Gd", "prod"], ["Gd"])
    V_(lambda e: e.tensor_copy(out=posI[:], in_=posk[:]), [K("posk", k) for k in range(4)], ["posI"])
    V_(lambda e: e.tensor_tensor(out=cmpb[:], in0=pend[:].unsqueeze(1).to_broadcast([128, NMB, E]),
                                 in1=iob[:].unsqueeze(2).to_broadcast([128, NMB, E]), op=ALU.is_le), ["pend", "iob"], ["cmpb"])
    V_(lambda e: e.tensor_reduce(out=blke[:], in_=cmpb[:], axis=AX.X, op=ALU.add), ["cmpb"], ["blke"])
    V_(lambda e: e.tensor_scalar(out=blke[:], in0=blke[:], scalar1=float(E - 1), scalar2=None, op0=ALU.min), ["blke"], ["blke"])
    V_(lambda e: e.scalar_tensor_tensor(out=idxf[:], in0=blke[:].unsqueeze(2).to_broadcast([128, NMB, 8]), scalar=float(D),
                                        in1=pk_f[:].unsqueeze(1).to_broadcast([128, NMB, 8]), op0=ALU.mult, op1=ALU.add),
       ["blke", "pk_f"], ["idxf"])
    V_(lambda e: e.tensor_copy(out=idxU[:], in_=idxf[:]), ["idxf"], ["idxU"])
    V_(lambda e: e.tensor_scalar(out=OH[:], in0=blke[0:32, :], scalar1=pid_f[0:32, 0:1], scalar2=None, op0=ALU.is_equal),
       ["blke", "pid_f"], ["OH"])
    for half in range(2):
        bk = P.next_bank()

        def bm(e, half=half, bk=bk):
            ins = None
            for q in range(8):
                fc = half * 8 + q
                ins = e.matmul(psf(bk)[:, q * NMB:(q + 1) * NMB], lhsT=bup_sb[0:32, fc * 128:(fc + 1) * 128], rhs=OH[0:32, :],
                               start=True, stop=True)
            return ins
        P.op("tensor", bm, reads=["bup_sb", "OH"], writes=[K("ps", bk)])
        if half == 0:
            V_(lambda e, bk=bk: e.tensor_copy(out=bupT[:, 0:8, :], in_=psf(bk)[:].rearrange("p (q b) -> p q b", b=NMB)),
               [K("ps", bk)], [K("bupT", 0)])
        else:
            V_(lambda e, bk=bk: e.tensor_scalar(out=bupT[:, 8:16, :], in0=psf(bk)[:].rearrange("p (q b) -> p q b", b=NMB),
                                               scalar1=1.0, scalar2=None, op0=ALU.add), [K("ps", bk)], [K("bupT", 1)])
    bupk = [K("bupT", 0), K("bupT", 1)]

    if True:
        checkpoint("route", [(posk[:].rearrange("p a b -> p (a b)"), 0), (gw[:].rearrange("p a b -> p (a b)"), 128), (blke[:], 256),
                             (tot[:], 320), (pend[:], 352), (bupT[:, 0, :], 384), (bupT[:, 8, :], 448),
                             (Gd[:].rearrange("p a b -> p (a b)"), 1024)])

    xbs = [rt.alloc(f"xbs{i}", [128, D], BF16)[0] for i in range(4)]
    xn2_t = xn2_scr.rearrange("(j p) d -> j p d", p=128)
    for j in range(NT):
        s = j % 4
        P.op("sync", lambda e, j=j, s=s: e.dma_start(out=xbs[s][:], in_=xn2_t[j]), writes=[K("xbs", s)], dma_sem=f"xbs{s}")
        P.op("gpsimd", lambda e, j=j, s=s: [e.indirect_dma_start(
            out=xg_scr, out_offset=IOA(ap=posI[:, k, j:j + 1], axis=0), in_=xbs[s][:], in_offset=None) for k in range(4)],
            reads=[K("xbs", s), "posI"], writes=[K("xg_scr", j)], dma_sem=f"xgs{s}", n_inc=4)
    P.barrier()

    WU = [ar.at(WT_off[2 * i], f"WU{i}", [128, 8, 2 * D], BF16) for i in range(2)]
    WD = [ar.at(WT_off[4 + i], f"WD{i}", [128, 8, D], BF16) for i in range(2)]
    mo = Arena(nc, moe_base, LIMIT)
    xg = [mo.alloc(f"xg{i}", [128, 4, D], BF16)[0] for i in range(2)]
    xgT2 = [mo.alloc(f"xgT{i}", [128, 8, 512], BF16)[0] for i in range(2)]
    actT, _ = mo.alloc("actT", [128, 8, 512], BF16)
    g1t = [mo.alloc(f"g1t{i}", [128, 512], F32)[0] for i in range(2)]
    sgt2 = [mo.alloc(f"sgt2{i}", [128, 512], BF16)[0] for i in range(2)]
    l1t = [mo.alloc(f"l1t{i}", [128, 512], BF16)[0] for i in range(2)]
    yo = [mo.alloc(f"yo{i}", [128, D], F32)[0] for i in range(2)]
    xg_v = xg_scr.rearrange("(b a p) d -> b p a d", a=4, p=128)
    yg_v = yg_scr.rearrange("(b a p) d -> b a p d", a=4, p=128)

    def moe_loads(b):
        s = b % 2
        P.op("gpsimd", lambda e, b=b, s=s: [e.indirect_dma_start(
            out=WU[s][:, ko, :], out_offset=None, in_=w_up, in_offset=IOA(ap=idxU[:, b, ko:ko + 1], axis=0)) for ko in range(8)],
            reads=["idxU"], writes=[K("WU", s)], dma_sem=f"WU{s}", n_inc=8)
        P.op("gpsimd", lambda e, b=b, s=s: [e.indirect_dma_start(
            out=WD[s][:, ko, :], out_offset=None, in_=w_down, in_offset=IOA(ap=idxU[:, b, ko:ko + 1], axis=0)) for ko in range(8)],
            reads=["idxU"], writes=[K("WD", s)], dma_sem=f"WD{s}", n_inc=8)
        P.op("sync", lambda e, b=b, s=s: e.dma_start(out=xg[s][:], in_=xg_v[b]), writes=[K("xg", s)], dma_sem=f"xg{s}")

    def moe_transposes(b):
        s = b % 2
        xgT = xgT2[s]
        for kp in range(4):
            bk = P.next_bank()

            def tr(e, kp=kp, bk=bk, s=s):
                ins = None
                for q in range(2):
                    ko = kp * 2 + q
                    for a in range(4):
                        ins = e.transpose(out=psb(bk)[:, q * 512 + a * 128:q * 512 + (a + 1) * 128],
                                          in_=xg[s][:, a, ko * 128:(ko + 1) * 128], identity=identb[:])
                return ins
            P.op("tensor", tr, reads=[K("xg", s), "identb"], writes=[K("ps", bk)])
            for q in range(2):
                ko = kp * 2 + q
                if kp % 2 == 0:
                    P.op("vector", lambda e, ko=ko, q=q, bk=bk: e.tensor_scalar(
                        out=xgT[:, ko, :], in0=psb(bk)[:, q * 512:(q + 1) * 512], scalar1=VT[:, ko, C_SC2:C_SC2 + 1],
                        scalar2=VT[:, ko, C_SH2:C_SH2 + 1], op0=ALU.mult, op1=ALU.add),
                        reads=[K("ps", bk)], writes=[K("xgT", s, ko)])
                else:
                    P.op("scalar", lambda e, ko=ko, q=q, bk=bk: e.activation(
                        out=xgT[:, ko, :], in_=psb(bk)[:, q * 512:(q + 1) * 512], func=AF.Identity,
                        bias=VT[:, ko, C_SH2:C_SH2 + 1], scale=VT[:, ko, C_SC2:C_SC2 + 1]),
                        reads=[K("ps", bk)], writes=[K("xgT", s, ko)])

    n_moe = NMB if moe_blocks is None else moe_blocks
    moe_loads(0)
    moe_transposes(0)
    ycount = 0
    for b in range(n_moe):
        s = b % 2
        xgT = xgT2[s]
        if b + 1 < n_moe:
            moe_loads(b + 1)
        WUk = [K("WU", s)]
        WDk = [K("WD", s)]
        xgTk = [K("xgT", s, ko) for ko in range(8)]
        for fc in range(8):
            bG, bL = P.next_bank(), P.next_bank()
            mm_group(bG, [(WU[s][:, ko, fc * 128:(fc + 1) * 128], xgT[:, ko, :]) for ko in range(8)], WUk + xgTk)
            mm_group(bL, [(WU[s][:, ko, D + fc * 128:D + (fc + 1) * 128], xgT[:, ko, :]) for ko in range(8)], WUk + xgTk)
            t = fc % 2
            V_(lambda e, fc=fc, b=b, bG=bG, t=t: e.tensor_scalar(out=g1t[t][:], in0=psf(bG)[:], scalar1=bupT[:, fc, b:b + 1], scalar2=SW_LIMIT,
                                                               op0=ALU.add, op1=ALU.min), [K("ps", bG)] + bupk, [K("g1t", t)])
            P.op("scalar", lambda e, t=t: e.activation(out=sgt2[t][:], in_=g1t[t][:], func=AF.Sigmoid, scale=SW_ALPHA),
                 reads=[K("g1t", t)], writes=[K("sgt2", t)])
            V_(lambda e, fc=fc, b=b, bL=bL, t=t: e.tensor_scalar(out=l1t[t][:], in0=psf(bL)[:], scalar1=bupT[:, 8 + fc, b:b + 1],
                                                               scalar2=1.0 - SW_LIMIT, op0=ALU.add, op1=ALU.max),
               [K("ps", bL)] + bupk, [K("l1t", t)])
            V_(lambda e, t=t: e.tensor_tensor(out=g1t[t][:], in0=g1t[t][:], in1=sgt2[t][:], op=ALU.mult),
               [K("g1t", t), K("sgt2", t)], [K("g1t", t)])
            V_(lambda e, fc=fc, t=t: e.scalar_tensor_tensor(out=actT[:, fc, :], in0=l1t[t][:], scalar=1.0 + SW_LIMIT, in1=g1t[t][:],
                                                           op0=ALU.min, op1=ALU.mult), [K("l1t", t), K("g1t", t)], [K("actT", fc)])
        actk = [K("actT", fc) for fc in range(8)]
        if b + 1 < n_moe:
            moe_transposes(b + 1)
        for a in range(4):
            ys_ = ycount % 2
            ycount += 1
            for nh in range(2):
                bk = P.next_bank()
                mm_group(bk, [(actT[:, fc, a * 128:(a + 1) * 128], WD[s][:, fc, nh * 512:(nh + 1) * 512]) for fc in range(8)], WDk + actk)
                V_(lambda e, nh=nh, bk=bk, ys_=ys_: e.tensor_tensor(out=yo[ys_][:, nh * 512:(nh + 1) * 512], in0=psf(bk)[:],
                                                                   in1=BC[:, BC_G2, nh * 512:(nh + 1) * 512], op=ALU.mult),
                   [K("ps", bk)], [K("yo", ys_, nh)])
            P.op("sync", lambda e, b=b, a=a, ys_=ys_: e.dma_start(out=yg_v[b][a], in_=yo[ys_][:]),
                 reads=[K("yo", ys_, 0), K("yo", ys_, 1)], writes=[K("yg", b, a)], dma_sem=f"yo{ys_}")
    P.barrier()

    cb = Arena(nc, WT_off[0], X_off)
    yk = [cb.alloc(f"yk{i}", [128, 4, D], F32)[0] for i in range(4)]
    xr = [cb.alloc(f"xr{i}", [128, D], F32)[0] for i in range(2)]
    acc = [cb.alloc(f"acc{i}", [128, D], F32)[0] for i in range(2)]
    gT = [cb.alloc(f"gT{i}", [32, 128], F32)[0] for i in range(2)]
    x1_t = x1_scr.rearrange("(j p) d -> j p d", p=128)
    out_t = out_d.rearrange("(j p) d -> j p d", p=128)
    def comb_g(j):
        g = j % 4
        P.op("gpsimd", lambda e, j=j, g=g: [e.indirect_dma_start(
            out=yk[g][:, k, :], out_offset=None, in_=yg_scr, in_offset=IOA(ap=posI[:, k, j:j + 1], axis=0)) for k in range(4)],
            reads=[], writes=[K("yk", g)], dma_sem=f"yk{g}", n_inc=4)

    def comb_a(j):
        s = j % 2
        g = j % 4
        P.op("sync", lambda e, j=j, s=s: e.dma_start(out=xr[s][:], in_=x1_t[j]), writes=[K("xr", s)], dma_sem=f"xr{s}")
        bk = P.next_bank()
        P.op("tensor", lambda e, j=j, bk=bk: e.transpose(out=psf(bk)[0:32, 0:128], in_=Gd[:, j, :], identity=identf[:]),
             reads=["identf"], writes=[K("ps", bk)])
        V_(lambda e, s=s, bk=bk: e.tensor_copy(out=gT[s][:], in_=psf(bk)[0:32, 0:128]), [K("ps", bk)], [K("gT", s)])
        for nh in range(2):
            bk = P.next_bank()
            mm_group(bk, [(gT[s][:], bdn_sb[0:32, nh * 512:(nh + 1) * 512])], [K("gT", s)])
            V_(lambda e, j=j, s=s, nh=nh, bk=bk, g=g: e.scalar_tensor_tensor(
                out=acc[s][:, nh * 512:(nh + 1) * 512], in0=yk[g][:, 0, nh * 512:(nh + 1) * 512], scalar=gw[:, j, 0:1], in1=psf(bk)[:],
                op0=ALU.mult, op1=ALU.add), [K("ps", bk), K("yk", g)], [K("acc", s, nh)])
        ak = [K("acc", s, 0), K("acc", s, 1)]
        for k in range(1, 4):
            V_(lambda e, j=j, s=s, k=k, g=g: e.scalar_tensor_tensor(out=acc[s][:], in0=yk[g][:, k, :], scalar=gw[:, j, k:k + 1], in1=acc[s][:],
                                                                   op0=ALU.mult, op1=ALU.add), ak + [K("yk", g)], ak)
        V_(lambda e, s=s: e.scalar_tensor_tensor(out=acc[s][:], in0=xr[s][:], scalar=ALPHA, in1=acc[s][:], op0=ALU.mult, op1=ALU.add),
           ak + [K("xr", s)], ak)

    def comb_b(j):
        s = j % 2
        ak = [K("acc", s, 0), K("acc", s, 1)]
        a4 = j % 4
        sk, rs, nbt = ln_stats4(lambda a, s=s: acc[s][:], j % 2, lambda a: ak, f"fin{j % 2}_", tiles=(a4,), use_act=True)
        P.op("scalar", lambda e, s=s, a4=a4, rs=rs, nbt=nbt: e.activation(out=acc[s][:], in_=acc[s][:], func=AF.Identity,
                                                                         bias=nbt[:, a4:a4 + 1], scale=rs[:, a4:a4 + 1]),
             reads=ak + sk, writes=ak)
        P.op("gpsimd", lambda e, s=s: e.tensor_tensor(out=acc[s][:], in0=acc[s][:], in1=BC[:, BC_P1G, :], op=ALU.mult), reads=ak, writes=ak)
        P.op("gpsimd", lambda e, s=s: e.tensor_tensor(out=acc[s][:], in0=acc[s][:], in1=BC[:, BC_P1B, :], op=ALU.add), reads=ak, writes=ak)
        P.op("sync", lambda e, j=j, s=s: e.dma_start(out=out_t[j], in_=acc[s][:]), reads=ak, writes=[K("out", j)], dma_sem=f"out{s}")

    for j in range(3):
        comb_g(j)
    comb_a(0)
    for j in range(NT):
        if j + 3 < NT:
            comb_g(j + 3)
        if j + 1 < NT:
            comb_a(j + 1)
        comb_b(j)
    P.barrier()
    P.emit()
    return nc


def _in_maps(inputs, stage=3):
    f = lambda a: np.ascontiguousarray(np.asarray(a, dtype=np.float32))
    g = {k: f(v) for k, v in inputs.items()}
    shared = {
        "w_ada": g["w_ada"][0], "b_ada": g["b_ada"][0].reshape(1, -1),
        "w_in": g["w_in"][0], "b_in": g["b_in"][0].reshape(6, D),
        "conv_w": g["conv_w"][0], "conv_b": g["conv_b"][0].reshape(1, D),
        "ln_a_g": g["ln_a_g"][0].reshape(1, D), "ln_a_b": g["ln_a_b"][0].reshape(1, D),
        "w_pa": g["w_pa"][0], "b_pa": g["b_pa"][0].reshape(1, D),
        "ln_v_g": g["ln_v_g"][0].reshape(1, D), "ln_v_b": g["ln_v_b"][0].reshape(1, D),
        "w_s": g["w_s"][0], "b_s": g["b_s"][0].reshape(1, D),
        "w_pb": g["w_pb"][0], "b_pb": g["b_pb"][0].reshape(1, D),
        "w_out": g["w_out"][0], "b_out": g["b_out"][0].reshape(1, D),
        "post1_g": g["post1_g"][0].reshape(1, D), "post1_b": g["post1_b"][0].reshape(1, D),
        "w_router": g["w_router"][0], "b_router": g["b_router"][0].reshape(1, E),
        "w_up": g["w_up"][0].reshape(E * D, 2 * D), "b_up": g["b_up"][0],
        "w_down": g["w_down"][0].reshape(E * D, D), "b_down": g["b_down"][0],
        "post2_g": g["post2_g"][0].reshape(1, D), "post2_b": g["post2_b"][0].reshape(1, D),
    }
    if stage < 2:
        for k in ("w_up", "b_up", "w_down", "b_down", "post2_g", "post2_b"):
            shared.pop(k)
    maps = []
    for b in range(8):
        m = dict(shared)
        m["x"] = g["x"][b]
        m["c"] = g["c"][b].reshape(1, D)
        maps.append(m)
    return maps


def kernel(**inputs):
    nc = build_program(stage=3)
    res = run_bass_kernel_spmd(nc, _in_maps(inputs), core_ids=list(range(8)))
    return np.stack([np.asarray(r["out"], dtype=np.float32) for r in res.results], axis=0)
```

```python
import contextlib
import numpy as np
import concourse.bass as bass
import concourse.mybir as mybir
from concourse.bass_utils import run_bass_kernel_spmd
from concourse.alu_op_type import AluOpType as ALU

F32 = mybir.dt.float32
BF16 = mybir.dt.bfloat16
I32 = mybir.dt.int32
AF = mybir.ActivationFunctionType
AX = mybir.AxisListType

T = 4096
D = 1024
NT = T // 128
NB = T // 512
E = 32
KW = 31
MB = 512
NMB = 64
ALPHA = 2.0 ** 0.25
EPS = 1e-5
SW_ALPHA = 1.702
SW_LIMIT = 7.0
ENGS = ("tensor", "vector", "scalar", "gpsimd", "sync")


class Prog:
    def __init__(self, nc):
        self.nc = nc
        self.ops = {e: [] for e in ENGS}
        self.sem_val = {}
        self.waited = {e: {} for e in ENGS}
        self.last_write = {}
        self.readers = {}
        self.bank = 0

    def _need(self, eng, tok, waits):
        if tok is None:
            return
        sem, val, teng = tok
        if teng == "tensor" and eng == "tensor":
            return
        if self.waited[eng].get(sem, 0) >= val:
            return
        if waits.get(sem, 0) < val:
            waits[sem] = val

    def op(self, eng, fn, reads=(), writes=(), dma_sem=None, n_inc=1):
        ps_reads = [r for r in reads if isinstance(r, tuple) and r and r[0] == "ps"]
        if ps_reads:
            reads = [r for r in reads if r not in ps_reads]
            writes = list(writes) + ps_reads
        waits = {}
        for r in reads:
            self._need(eng, self.last_write.get(r), waits)
        for w in writes:
            self._need(eng, self.last_write.get(w), waits)
            for t in self.readers.get(w, {}).values():
                self._need(eng, t, waits)
        for s, v in waits.items():
            self.waited[eng][s] = v
        if dma_sem is not None:
            sem, amt, teng = "D_" + dma_sem, 16, "dma"
        else:
            sem, amt, teng = "E_" + eng, 1, eng
        val = self.sem_val.get(sem, 0) + amt * n_inc
        self.sem_val[sem] = val
        tok = (sem, val, teng)
        for r in reads:
            self.readers.setdefault(r, {})[sem] = tok
        for w in writes:
            self.last_write[w] = tok
            self.readers[w] = {}
        self.ops[eng].append((fn, list(waits.items()), (sem, amt)))
        return tok

    def barrier(self):
        for eng in ENGS:
            waits = []
            for s, v in self.sem_val.items():
                if self.waited[eng].get(s, 0) < v:
                    waits.append((s, v))
                    self.waited[eng][s] = v
            self.ops[eng].append((None, waits, None))
        self.last_write = {}
        self.readers = {}

    def alias(self, new_keys, old_keys):
        toks = {}
        for k in old_keys:
            t = self.last_write.get(k)
            if t is not None:
                if toks.get(t[0], (None, 0))[1] < t[1]:
                    toks[t[0]] = t
            for t in self.readers.get(k, {}).values():
                if toks.get(t[0], (None, 0))[1] < t[1]:
                    toks[t[0]] = t
        for k in new_keys:
            self.last_write[k] = None
            self.readers[k] = dict(toks)

    def next_bank(self):
        b = self.bank
        self.bank = (b + 1) % 8
        return b

    def emit(self):
        nc = self.nc
        names = sorted(self.sem_val.keys())
        with contextlib.ExitStack() as st:
            sems = {n: st.enter_context(nc.semaphore(n)) for n in names}
            block = st.enter_context(nc.Block())

            def mk(engname):
                def body(e):
                    for fn, waits, inc in self.ops[engname]:
                        for s, v in waits:
                            e.wait_ge(sems[s], v)
                        if fn is None:
                            continue
                        ins = fn(e)
                        for i_ in (ins if isinstance(ins, list) else [ins]):
                            i_.then_inc(sems[inc[0]], inc[1])
                return body

            block.tensor(mk("tensor"))
            block.vector(mk("vector"))
            block.scalar(mk("scalar"))
            block.gpsimd(mk("gpsimd"))
            block.sync(mk("sync"))


class Arena:
    def __init__(self, nc, base, limit):
        self.nc, self.ptr, self.limit, self.n = nc, base, limit, 0

    def at(self, off, name, shape, dt):
        self.n += 1
        return self.nc.alloc_sbuf_tensor_at(f"{name}_{self.n}", list(shape), dt, offset=off)

    def alloc(self, name, shape, dt, nbytes=None):
        esz = 2 if dt == BF16 else 4
        if nbytes is None:
            nbytes = int(np.prod(shape[1:])) * esz
        nbytes = (nbytes + 31) // 32 * 32
        off = self.ptr
        self.ptr += nbytes
        assert self.ptr <= self.limit, (name, self.ptr, self.limit)
        return self.at(off, name, shape, dt), off


def build_program(stage=3, stop=None, moe_blocks=None):
    try:
        return _build_program(stage, stop, moe_blocks)
    except _StopBuild as s:
        return s.nc


class _StopBuild(Exception):
    def __init__(self, nc):
        self.nc = nc


def _build_program(stage, stop, moe_blocks):
    nc = bass.Bass("TRN2", target_bir_lowering=False)
    P = Prog(nc)

    def din(name, shape, dt=F32):
        return nc.dram_tensor(name, list(shape), dt, kind="ExternalInput").ap()

    x_d = din("x", [T, D])
    c_d = din("c", [1, D])
    w_ada = din("w_ada", [D, 6 * D]); b_ada = din("b_ada", [1, 6 * D])
    w_in = din("w_in", [D, 6 * D]); b_in = din("b_in", [6, D])
    conv_w = din("conv_w", [KW, D]); conv_b = din("conv_b", [1, D])
    ln_a_g = din("ln_a_g", [1, D]); ln_a_b = din("ln_a_b", [1, D])
    w_pa = din("w_pa", [D, D]); b_pa = din("b_pa", [1, D])
    ln_v_g = din("ln_v_g", [1, D]); ln_v_b = din("ln_v_b", [1, D])
    w_s = din("w_s", [8, 128, 128]); b_s = din("b_s", [1, D])
    w_pb = din("w_pb", [D, D]); b_pb = din("b_pb", [1, D])
    w_out = din("w_out", [D, D]); b_out = din("b_out", [1, D])
    post1_g = din("post1_g", [1, D]); post1_b = din("post1_b", [1, D])
    w_router = din("w_router", [D, E]); b_router = din("b_router", [1, E])
    if stage >= 2:
        w_up = din("w_up", [E * D, 2 * D]); b_up = din("b_up", [E, 2 * D])
        w_down = din("w_down", [E * D, D]); b_down = din("b_down", [E, D])
        post2_g = din("post2_g", [1, D]); post2_b = din("post2_b", [1, D])

    okind = "ExternalOutput"
    out_d = nc.dram_tensor("out", [T, D], F32, kind=okind).ap()
    dbg = stage < 3
    x1_scr = nc.dram_tensor("x1_scr", [T, D], F32, kind=okind if dbg else "Internal").ap()
    xn2_scr = nc.dram_tensor("xn2_scr", [T, D], BF16, kind="Internal").ap()
    wbf = nc.dram_tensor("wbf", [9, 128, 8 * D], BF16, kind="Internal").ap()
    lg_scr = nc.dram_tensor("lg_scr", [128, NT * E], F32, kind=okind if dbg else "Internal").ap()
    xg_scr = nc.dram_tensor("xg_scr", [NMB * MB, D], BF16, kind="Internal").ap()
    yg_scr = nc.dram_tensor("yg_scr", [NMB * MB, D], F32, kind="Internal").ap()

    KB = 1024
    LIMIT = 229376
    ar = Arena(nc, 16640, LIMIT)
    identb, _ = ar.alloc("identb", [128, 128], BF16)
    identf, _ = ar.alloc("identf", [128, 128], F32)
    onesb, _ = ar.alloc("onesb", [128, 128], BF16)
    onesf, _ = ar.alloc("onesf", [128, 128], F32)
    VT, _ = ar.alloc("VT", [128, 8, 50], F32)
    cond, _ = ar.alloc("cond", [128, 8], F32)
    WcT, _ = ar.alloc("WcT", [128, 8, 128], BF16)
    bs_hl, _ = ar.alloc("bs_hl", [33, D], BF16)
    bv_hl, _ = ar.alloc("bv_hl", [33, D], BF16)
    bo_hl, _ = ar.alloc("bo_hl", [33, D], BF16)
    wr, _ = ar.alloc("wr", [128, 8, E], F32)
    br_row, _ = ar.alloc("br_row", [1, E], F32)
    mhalf, _ = ar.alloc("mhalf", [128, 2], F32)
    ztile, _ = ar.alloc("ztile", [128, D], BF16)
    epsc, _ = ar.alloc("epsc", [128, 2], F32)
    BC, _ = ar.alloc("BC", [128, 6, D], F32)
    BC_P1G, BC_P1B, BC_LVG, BC_LVB, BC_G1, BC_G2 = range(6)
    STS = []
    for i in range(4):
        s6, _ = ar.alloc(f"st6_{i}", [128, 4, 2, 6], F32)
        m2, _ = ar.alloc(f"mv_{i}", [128, 4, 2], F32)
        r1, _ = ar.alloc(f"rstd_{i}", [128, 4], F32)
        n1, _ = ar.alloc(f"nb_{i}", [128, 4], F32)
        STS.append((s6, m2, r1, n1))
    base = ar.ptr
    WT_off = [base + i * 16 * KB for i in range(6)]
    X_off = base + 96 * KB
    X_size = LIMIT - X_off

    ps = [nc.alloc_psum_tensor(f"ps{i}", [128, 512], F32) for i in range(8)]

    def psf(i):
        return ps[i]

    def psb(i):
        return ps[i][:].bitcast(BF16)

    K = lambda *a: tuple(a)

    dbg_d = nc.dram_tensor("dbg", [128, 8192], F32, kind="ExternalOutput").ap() if stop is not None else None

    def checkpoint(k, dumps=()):
        if stop != k:
            return
        i = 0
        for (ap, c0) in dumps:
            n = ap.shape[-1]
            for s0 in range(0, n, 1024):
                s1 = min(n, s0 + 1024)
                P.op("gpsimd", lambda e, ap=ap, c0=c0, s0=s0, s1=s1: e.dma_start(
                    out=dbg_d[0:ap.shape[0], c0 + s0:c0 + s1], in_=ap[:, s0:s1]),
                    reads=list(P.last_write.keys()), writes=[K("dbg", i)], dma_sem=f"dbg{i}")
                i += 1
        P.barrier()
        P.emit()
        raise _StopBuild(nc)

    def mm_group(bk, pairs, reads, out_ap=None):
        out_ap = psf(bk)[:] if out_ap is None else out_ap

        def fn(e):
            ins = None
            n = len(pairs)
            for i, (l, r) in enumerate(pairs):
                ins = e.matmul(out_ap, lhsT=l, rhs=r, start=(i == 0), stop=(i == n - 1))
            return ins
        P.op("tensor", fn, reads=reads, writes=[K("ps", bk)])

    su = Arena(nc, base, LIMIT)
    V, _ = su.alloc("V", [50, D], F32)
    V2, _ = su.alloc("V2", [6, D], F32)
    io_i, _ = su.alloc("io_i", [128, 128], I32)
    condrep, _ = su.alloc("condrep", [128, 8, 128], F32)
    badarow, _ = su.alloc("badarow", [1, 6 * D], F32)
    modbc, _ = su.alloc("modbc", [128, 6 * D], F32)
    wa = [su.alloc(f"wa{i}", [128, 8, 512], F32)[0] for i in range(2)]
    wsld, _ = su.alloc("wsld", [128, 8, 128], F32)
    wsT, _ = su.alloc("wsT", [128, 8, 128], F32)
    brow, _ = su.alloc("brow", [33, D], F32)
    brow2, _ = su.alloc("brow2", [33, D], F32)
    bhi, _ = su.alloc("bhi", [33, D], BF16)

    P.op("gpsimd", lambda e: e.iota(io_i[:], pattern=[[1, 128]], base=0, channel_multiplier=-1), writes=["io_i"])
    P.op("vector", lambda e: e.tensor_scalar(out=identb[:], in0=io_i[:], scalar1=0, scalar2=None, op0=ALU.is_equal),
         reads=["io_i"], writes=["identb"])
    P.op("vector", lambda e: e.tensor_scalar(out=identf[:], in0=io_i[:], scalar1=0, scalar2=None, op0=ALU.is_equal),
         reads=["io_i"], writes=["identf"])
    P.op("gpsimd", lambda e: e.memset(onesb[:], 1.0), writes=["onesb"])
    P.op("gpsimd", lambda e: e.memset(onesf[:], 1.0), writes=["onesf"])
    P.op("gpsimd", lambda e: e.memset(mhalf[:], -0.5), writes=["mhalf"])
    P.op("gpsimd", lambda e: e.memset(epsc[:], EPS), writes=["epsc"])
    P.op("gpsimd", lambda e: e.memset(bs_hl[:], 0.0), writes=["bs_hl"])
    P.op("gpsimd", lambda e: e.memset(bv_hl[:], 0.0), writes=["bv_hl"])
    P.op("gpsimd", lambda e: e.memset(bo_hl[:], 0.0), writes=["bo_hl"])
    P.op("gpsimd", lambda e: e.memset(V[:], 0.0), writes=["Vz"])

    vrows = [(b_in, 0, 6), (conv_b, 6, 1), (ln_a_g, 7, 1), (ln_a_b, 8, 1), (b_pa, 9, 1), (b_pb, 10, 1),
             (c_d, 11, 1), (conv_w, 12, KW)]
    for i, (src, r0, n) in enumerate(vrows):
        P.op("sync", lambda e, src=src, r0=r0, n=n: e.dma_start(out=V[r0:r0 + n, :], in_=src),
             reads=["Vz"], writes=[K("V", i)], dma_sem=f"V{i}")
    vkeys = [K("V", i) for i in range(len(vrows))]

    def vt_transposes(src, nrows, col0, rkeys, wkey):
        for half in range(2):
            bk = P.next_bank()

            def tr(e, half=half, bk=bk):
                ins = None
                for q in range(4):
                    ch = half * 4 + q
                    ins = e.transpose(out=psf(bk)[:, q * 64:q * 64 + nrows], in_=src[0:nrows, ch * 128:(ch + 1) * 128],
                                      identity=identf[0:nrows, 0:nrows])
                return ins
            P.op("tensor", tr, reads=list(rkeys) + ["identf"], writes=[K("ps", bk)])
            P.op("vector", lambda e, half=half, bk=bk: e.tensor_copy(
                out=VT[:, half * 4:half * 4 + 4, col0:col0 + nrows],
                in_=psf(bk)[:, 0:256].rearrange("p (q r) -> p q r", q=4)[:, :, 0:nrows]),
                reads=[K("ps", bk)], writes=[K(wkey, half)])

    vt_transposes(V, 44, 0, vkeys, "VT")
    VTK = [K("VT", 0), K("VT", 1)]
    cv = [su.alloc(f"cv{i}", [128, 8, D], BF16)[0] for i in range(2)]
    w_in_v0 = w_in.rearrange("(ko p) n -> p ko n", p=128)
    chunks = [
        [(0, 512, w_in_v0[:, :, 0:512]), (512, 512, w_in_v0[:, :, D:D + 512])],
        [(0, 512, w_in_v0[:, :, 512:D]), (512, 512, w_in_v0[:, :, D + 512:2 * D])],
        [(0, D, w_in_v0[:, :, 2 * D:3 * D])], [(0, D, w_in_v0[:, :, 3 * D:4 * D])],
        [(0, D, w_in_v0[:, :, 4 * D:5 * D])], [(0, D, w_in_v0[:, :, 5 * D:6 * D])],
        [(0, D, w_pa.rearrange("(ko p) n -> p ko n", p=128))], [(0, D, w_pb.rearrange("(ko p) n -> p ko n", p=128))],
        [(0, D, w_out.rearrange("(ko p) n -> p ko n", p=128))],
    ]
    def convert_chunk(ci):
        parts = chunks[ci]
        s = ci % 2
        if ci < 8:
            dst = wbf[ci].rearrange("p (k n) -> p k n", k=8)
            for pi, (c0, n, srcap) in enumerate(parts):
                P.op("gpsimd", lambda e, dst=dst, c0=c0, n=n, srcap=srcap: e.dma_start(out=dst[:, :, c0:c0 + n], in_=srcap),
                     writes=[K("wbf", ci, pi)], dma_sem="wbfd")
            return
        for pi, (c0, n, srcap) in enumerate(parts):
            P.op("gpsimd", lambda e, s=s, c0=c0, n=n, srcap=srcap: e.dma_start(out=cv[s][:, :, c0:c0 + n], in_=srcap),
                 writes=[K("cv", s, pi)], dma_sem=f"cv{s}_{pi}")
        ck_ = [K("cv", s, pi) for pi in range(len(parts))]
        for h in range(2):
            P.op("vector", lambda e, s=s, h=h: e.tensor_tensor(
                out=cv[s][:, :, h * 512:(h + 1) * 512], in0=cv[s][:, :, h * 512:(h + 1) * 512],
                in1=BC[:, BC_G1, h * 512:(h + 1) * 512].unsqueeze(1).to_broadcast([128, 8, 512]), op=ALU.mult),
                reads=ck_ + [K("BC", BC_G1)], writes=ck_)
        P.op("gpsimd", lambda e, s=s, ci=ci: e.dma_start(out=wbf[ci], in_=cv[s][:].rearrange("p k n -> p (k n)")),
             reads=ck_, writes=[K("wbf", ci)], dma_sem=f"wbf{s}")

    for ci in range(8):
        convert_chunk(ci)
    P.op("scalar", lambda e: e.activation(out=cond[:], in_=VT[:, :, 11], func=AF.Silu), reads=VTK, writes=["cond"])
    for ko in range(8):
        P.op("vector", lambda e, ko=ko: e.tensor_scalar(out=condrep[:, ko, :], in0=onesf[:], scalar1=cond[:, ko:ko + 1],
                                                        scalar2=None, op0=ALU.mult),
             reads=["cond", "onesf"], writes=[K("condrep", ko)])
    P.op("sync", lambda e: e.dma_start(out=badarow[:], in_=b_ada), writes=["badarow"], dma_sem="badarow")
    w_ada_v = w_ada.rearrange("(ko p) n -> p ko n", p=128)
    for cc in range(12):
        s = cc % 2
        P.op("sync", lambda e, cc=cc, s=s: e.dma_start(out=wa[s][:], in_=w_ada_v[:, :, cc * 512:(cc + 1) * 512]),
             writes=[K("wa", s)], dma_sem=f"wa{s}")
        bk = P.next_bank()
        mm_group(bk, [(condrep[:, ko, :], wa[s][:, ko, :]) for ko in range(8)]
                 + [(onesf[0:1, :], badarow[0:1, cc * 512:(cc + 1) * 512])],
                 [K("wa", s), "badarow", "onesf"] + [K("condrep", ko) for ko in range(8)])
        P.op("scalar", lambda e, cc=cc, bk=bk: e.copy(out=modbc[:, cc * 512:(cc + 1) * 512], in_=psf(bk)[:]),
             reads=[K("ps", bk)], writes=[K("modbc", cc)])
    mkeys = [K("modbc", cc) for cc in range(12)]
    P.op("vector", lambda e: e.tensor_copy(out=BC[:, BC_G1, :], in_=modbc[:, 2 * D:3 * D]), reads=mkeys, writes=[K("BC", BC_G1)])
    P.op("vector", lambda e: e.tensor_copy(out=BC[:, BC_G2, :], in_=modbc[:, 5 * D:6 * D]), reads=mkeys, writes=[K("BC", BC_G2)])
    for i in range(6):
        P.op("sync", lambda e, i=i: e.dma_start(out=V2[i:i + 1, :], in_=modbc[i:i + 1, i * D:(i + 1) * D]),
             reads=mkeys, writes=[K("V2", i)], dma_sem=f"V2_{i}")
    vt_transposes(V2, 6, 44, [K("V2", i) for i in range(6)], "VTm")
    VTK = VTK + [K("VTm", 0), K("VTm", 1)]
    C_SH1, C_SC1, C_SH2, C_SC2 = 44, 45, 47, 48
    for col in (C_SC1, C_SC2):
        P.op("vector", lambda e, col=col: e.tensor_scalar(out=VT[:, :, col], in0=VT[:, :, col], scalar1=1.0, scalar2=None, op0=ALU.add),
             reads=VTK, writes=VTK)
    for slot, src in ((BC_P1G, post1_g), (BC_P1B, post1_b), (BC_LVG, ln_v_g), (BC_LVB, ln_v_b)):
        P.op("sync", lambda e, slot=slot, src=src: e.dma_start(out=BC[:, slot, :], in_=src.to_broadcast([128, D])),
             writes=[K("BC", slot)], dma_sem=f"BC{slot}")
    P.op("sync", lambda e: e.dma_start(out=wsld[:], in_=w_s.rearrange("h t s -> t h s")), writes=["wsld"], dma_sem="wsld")
    for half in range(2):
        bk = P.next_bank()

        def tr(e, half=half, bk=bk):
            ins = None
            for q in range(4):
                ins = e.transpose(out=psf(bk)[:, q * 128:(q + 1) * 128], in_=wsld[:, half * 4 + q, :], identity=identf[:])
            return ins
        P.op("tensor", tr, reads=["wsld", "identf"], writes=[K("ps", bk)])
        P.op("vector", lambda e, half=half, bk=bk: e.tensor_copy(
            out=wsT[:, half * 4:half * 4 + 4, :], in_=psf(bk)[:].rearrange("p (q t) -> p q t", q=4)),
            reads=[K("ps", bk)], writes=[K("wsT", half)])
    P.op("gpsimd", lambda e: e.affine_select(out=WcT[:], in_=wsT[:], pattern=[[0, 8], [1, 128]], compare_op=ALU.is_ge,
                                             fill=0.0, base=0, channel_multiplier=-1),
         reads=[K("wsT", 0), K("wsT", 1)], writes=["WcT"])

    def hilo(dst, dkey, prep):
        for row in (0, 32):
            prep(row)
        P.op("vector", lambda e: e.tensor_copy(out=dst[0:1, :], in_=brow[0:1, :]), reads=["brow0"], writes=[dkey])
        P.op("vector", lambda e: e.tensor_copy(out=bhi[32:33, :], in_=brow[32:33, :]), reads=["brow32"], writes=["bhi"])
        P.op("vector", lambda e: e.tensor_tensor(out=dst[32:33, :], in0=brow[32:33, :], in1=bhi[32:33, :], op=ALU.subtract),
             reads=["brow32", "bhi"], writes=[dkey])

    def load_row(src):
        def prep(row):
            P.op("sync", lambda e: e.dma_start(out=brow[row:row + 1, :], in_=src), writes=[f"brow{row}"], dma_sem=f"brow{row}")
        return prep
    hilo(bs_hl, "bs_hl", load_row(b_s))
    hilo(bv_hl, "bv_hl", load_row(b_in[3:4, :]))

    def prep_bo(row):
        P.op("sync", lambda e: e.dma_start(out=brow2[row:row + 1, :], in_=b_out), writes=[f"brow2_{row}"], dma_sem=f"brow2_{row}")
        P.op("vector", lambda e: e.tensor_tensor(out=brow[row:row + 1, :], in0=brow2[row:row + 1, :], in1=BC[row:row + 1, BC_G1, :],
                                                 op=ALU.mult),
             reads=[f"brow2_{row}", K("BC", BC_G1)], writes=[f"brow{row}"])
    hilo(bo_hl, "bo_hl", prep_bo)
    convert_chunk(8)
    P.op("sync", lambda e: e.dma_start(out=wr[:], in_=w_router.rearrange("(ko p) e -> p ko e", p=128)), writes=["wr"], dma_sem="wr")
    P.op("sync", lambda e: e.dma_start(out=br_row[:], in_=b_router), writes=["br_row"], dma_sem="br_row")
    checkpoint("setup", [(VT[:].rearrange("p a b -> p (a b)"), 0), (BC[:, BC_G1, :], 512), (BC[:, BC_P1G, :], 1536),
                         (WcT[:].rearrange("p a b -> p (a b)"), 2560), (bs_hl[:], 3584), (bo_hl[:], 4608), (cond[:], 5632)])

    P.barrier()

    WS = [ar.at(WT_off[i], f"WS{i}", [128, 8, D], BF16) for i in range(2)]
    A_off = [WT_off[2] + i * 8 * KB for i in range(8)] + [X_off + 16 * KB]
    Afm = [ar.at(o, "Afm", [128, 8, 512], BF16) for o in A_off]
    Atm = [ar.at(o, "Atm", [128, 4, D], BF16) for o in A_off]
    F1 = ar.at(X_off, "F1", [128, 4, D], F32)
    mx = Arena(nc, X_off + 24 * KB, LIMIT)
    zA, _ = mx.alloc("zA", [128, 8, 544], BF16)
    DG = [mx.alloc(f"DG{i}", [128, KW, 128], BF16)[0] for i in range(2)]
    sgt = [mx.alloc(f"sgt{i}", [128, 512], BF16)[0] for i in range(2)]
    tf = [mx.alloc(f"tf{i}", [128, 512], F32)[0] for i in range(2)]
    tfn = [mx.alloc(f"tfn{i}", [128, 512], F32)[0] for i in range(2)]
    xn2f = [mx.alloc(f"xn2f{i}", [128, D], F32)[0] for i in range(2)]
    h2T1, _ = mx.alloc("h2T", [128, 8, 128], F32)
    h2T = [h2T1, h2T1]
    lgs = [mx.alloc(f"lgs{i}", [128, E], F32)[0] for i in range(2)]

    A_XN = A_GATED = 0
    A_HT, A_GU, A_V = 1, 2, 3
    A_ZC = A_TA = 4
    A_SQ = A_XN2 = 5
    A_ZS, A_SGA, A_SGB = 6, 7, 8
    xn_t, hT, gu, vtok = Atm[A_XN], Afm[A_HT], Afm[A_GU], Atm[A_V]
    zc, sq, zs, gated = Afm[A_ZC], Afm[A_SQ], Afm[A_ZS], Afm[A_GATED]
    sgA, sgB, tA, xn2b = Afm[A_SGA], Afm[A_SGB], Afm[A_TA], Atm[A_XN2]
    merged = tA
    xnk = [K("xn", a) for a in range(4)]
    gatedk = [K("gated", a, hh) for a in range(4) for hh in range(2)]
    zck = [K("zc", fc) for fc in range(8)]
    tAk = [K("tA", fc) for fc in range(8)]
    sqk = [K("sq", fc) for fc in range(8)]
    xn2bk = [K("xn2b", a) for a in range(4)]

    x_v = x_d.rearrange("(b a p) d -> b p a d", a=4, p=128)
    x1_v = x1_scr.rearrange("(b a p) d -> b p a d", a=4, p=128)
    xn2_v = xn2_scr.rearrange("(b a p) d -> b p a d", a=4, p=128)
    w_in_v = w_in.rearrange("(ko p) n -> p ko n", p=128)
    w_pa_v = w_pa.rearrange("(ko p) n -> p ko n", p=128)
    w_pb_v = w_pb.rearrange("(ko p) n -> p ko n", p=128)
    w_out_v = w_out.rearrange("(ko p) n -> p ko n", p=128)

    P.op("gpsimd", lambda e: e.memset(zA[:], 0.0), writes=[K("zA", fc) for fc in range(8)])
    if stage >= 2:
        P.op("gpsimd", lambda e: e.memset(ztile[:], 0.0), writes=["ztile"])
        xg_z = xg_scr.rearrange("(p c r) d -> c p r d", p=128, c=16)

    wcount = [0]

    def load_w(ci):
        s = wcount[0] % 2
        wcount[0] += 1
        P.op("sync", lambda e, s=s, ci=ci: e.dma_start(out=WS[s][:].rearrange("p k n -> p (k n)"), in_=wbf[ci]),
             writes=[K("W", s, 0), K("W", s, 1)], dma_sem=f"W{s}")
        return s

    def wkeys(s):
        return [K("W", s, 0), K("W", s, 1)]

    def ln_tile_stats(src_fn, sset, rkeys, tag, a):
        st, mvt, rs, nbt = STS[sset]
        for h in range(2):
            P.op("vector", lambda e, a=a, h=h: e.bn_stats(out=st[:, a, h, :], in_=src_fn(a)[:, h * 512:(h + 1) * 512]),
                 reads=rkeys, writes=[K(tag + "st", a, h)])
        P.op("vector", lambda e, a=a: e.bn_aggr(out=mvt[:, a, :], in_=st[:, a, :, :].rearrange("p h s -> p (h s)")),
             reads=[K(tag + "st", a, 0), K(tag + "st", a, 1)], writes=[K(tag + "mv", a)])

    def ln_finish(sset, tag, tiles=(0, 1, 2, 3), use_act=False):
        st, mvt, rs, nbt = STS[sset]
        a0, a1 = tiles[0], tiles[-1] + 1
        mvk = [K(tag + "mv", a) for a in tiles]
        if use_act:
            P.op("scalar", lambda e: e.activation(out=rs[:, a0:a1], in_=mvt[:, a0:a1, 1], func=AF.Sqrt, bias=epsc[:, 0:1], scale=1.0),
                 reads=mvk, writes=[tag + "rs"])
            P.op("vector", lambda e: e.reciprocal(out=rs[:, a0:a1], in_=rs[:, a0:a1]), reads=[tag + "rs"], writes=[tag + "rs"])
        else:
            P.op("vector", lambda e: e.tensor_scalar(out=rs[:, a0:a1], in0=mvt[:, a0:a1, 1], scalar1=EPS, scalar2=None, op0=ALU.add),
                 reads=mvk, writes=[tag + "rs"])
            P.op("gpsimd", lambda e: e.tensor_tensor(out=rs[:, a0:a1], in0=rs[:, a0:a1], in1=mhalf[:, 0:1].to_broadcast([128, a1 - a0]),
                                                     op=ALU.pow), reads=[tag + "rs"], writes=[tag + "rs"])
        P.op("vector", lambda e: e.scalar_tensor_tensor(out=nbt[:, a0:a1], in0=mvt[:, a0:a1, 0], scalar=-1.0, in1=rs[:, a0:a1],
                                                        op0=ALU.mult, op1=ALU.mult),
             reads=mvk + [tag + "rs"], writes=[tag + "nb"])
        return [tag + "rs", tag + "nb"], rs, nbt

    def ln_stats4(src_fn, sset, rkeys_fn, tag, tiles=(0, 1, 2, 3), use_act=False):
        for a in tiles:
            ln_tile_stats(src_fn, sset, rkeys_fn(a), tag, a)
        return ln_finish(sset, tag, tiles, use_act)

    def head_tile(tb, a):
        P.op("sync", lambda e, tb=tb, a=a: e.dma_start(out=F1[:, a, :], in_=x_v[tb][:, a, :]), writes=[K("F1", a)], dma_sem=f"F1_{a}")
        ln_tile_stats(lambda a: F1[:, a, :], 0, [K("F1", a)], "l1", a)

    for tb in range(NB):
        if tb == 0:
            for a in range(4):
                head_tile(0, a)
        P.alias(xnk, gatedk)
        sk, rs, nbt = ln_finish(0, "l1")
        for a in range(4):
            P.op("scalar", lambda e, a=a, rs=rs, nbt=nbt: e.activation(out=xn_t[:, a, :], in_=F1[:, a, :], func=AF.Identity,
                                                                       bias=nbt[:, a:a + 1], scale=rs[:, a:a + 1]),
                 reads=[K("F1", a)] + sk, writes=[K("xn", a)])
        if tb == 0:
            checkpoint("ln1", [(xn_t[:, 0, :], 0), (xn_t[:, 3, :], 1024)])
        for kp in range(4):
            bk = P.next_bank()

            def tr(e, kp=kp, bk=bk):
                ins = None
                for q in range(2):
                    ko = kp * 2 + q
                    for a in range(4):
                        ins = e.transpose(out=psb(bk)[:, q * 512 + a * 128:q * 512 + (a + 1) * 128],
                                          in_=xn_t[:, a, ko * 128:(ko + 1) * 128], identity=identb[:])
                return ins
            P.op("tensor", tr, reads=xnk + ["identb"], writes=[K("ps", bk)])
            for q in range(2):
                ko = kp * 2 + q
                if kp % 2 == 0:
                    P.op("vector", lambda e, ko=ko, q=q, bk=bk: e.tensor_scalar(
                        out=hT[:, ko, :], in0=psb(bk)[:, q * 512:(q + 1) * 512], scalar1=VT[:, ko, C_SC1:C_SC1 + 1],
                        scalar2=VT[:, ko, C_SH1:C_SH1 + 1], op0=ALU.mult, op1=ALU.add),
                        reads=[K("ps", bk)] + VTK, writes=[K("hT", ko)])
                else:
                    P.op("scalar", lambda e, ko=ko, q=q, bk=bk: e.activation(
                        out=hT[:, ko, :], in_=psb(bk)[:, q * 512:(q + 1) * 512], func=AF.Identity,
                        bias=VT[:, ko, C_SH1:C_SH1 + 1], scale=VT[:, ko, C_SC1:C_SC1 + 1]),
                        reads=[K("ps", bk)] + VTK, writes=[K("hT", ko)])
        hTk = [K("hT", ko) for ko in range(8)]
        if tb == 0:
            checkpoint("xn", [(xn_t[:, 0, :], 0), (xn_t[:, 3, :], 1024)])
            checkpoint("hT", [(hT[:].rearrange("p a b -> p (a b)"), 0)])

        for t2 in range(2):
            s = load_w(t2)
            for c4 in range(4):
                fc = t2 * 4 + c4
                bv, bg = P.next_bank(), P.next_bank()
                mm_group(bv, [(WS[s][:, ko, c4 * 128:(c4 + 1) * 128], hT[:, ko, :]) for ko in range(8)], wkeys(s) + hTk)
                mm_group(bg, [(WS[s][:, ko, 512 + c4 * 128:512 + (c4 + 1) * 128], hT[:, ko, :]) for ko in range(8)], wkeys(s) + hTk)
                sg = sgt[fc % 2]
                P.op("scalar", lambda e, fc=fc, bg=bg, sg=sg: e.activation(out=sg[:], in_=psf(bg)[:], func=AF.Sigmoid,
                                                                         bias=VT[:, fc, 1:2], scale=1.0),
                     reads=[K("ps", bg)] + VTK, writes=[K("sgt", fc % 2)])
                P.op("vector", lambda e, fc=fc, bv=bv, sg=sg: e.scalar_tensor_tensor(
                    out=zA[:, fc, 30:542], in0=psf(bv)[:], scalar=VT[:, fc, 0:1], in1=sg[:], op0=ALU.add, op1=ALU.mult),
                    reads=[K("ps", bv), K("sgt", fc % 2)] + VTK, writes=[K("zA", fc)])
        if tb == 0:
            checkpoint("glu", [(zA[:, fc, 30:542], fc * 512) for fc in range(8)])
        if stage >= 2:
            for c in (2 * tb, 2 * tb + 1):
                P.op("gpsimd", lambda e, c=c: e.dma_start(out=xg_z[c], in_=ztile[:].unsqueeze(1).to_broadcast([128, 16, D])),
                     reads=["ztile"], writes=[K("xgz", c)], dma_sem="xgz")
        s = load_w(2)
        for fc in range(8):
            bk = P.next_bank()
            mm_group(bk, [(WS[s][:, ko, fc * 128:(fc + 1) * 128], hT[:, ko, :]) for ko in range(8)], wkeys(s) + hTk)
            P.op("scalar", lambda e, fc=fc, bk=bk: e.activation(out=gu[:, fc, :], in_=psf(bk)[:], func=AF.Gelu,
                                                                bias=VT[:, fc, 2:3], scale=1.0),
                 reads=[K("ps", bk)] + VTK, writes=[K("gu", fc)])
        if tb == 0:
            checkpoint("gu", [(gu[:].rearrange("p a b -> p (a b)"), 0)])
        P.alias(zck, tAk)
        P.alias(sqk, xn2bk)
        for fc in range(8):
            dg = DG[fc % 2]
            P.op("vector", lambda e, fc=fc, dg=dg: e.tensor_tensor(
                out=dg[:], in0=identb[:].unsqueeze(1).to_broadcast([128, KW, 128]),
                in1=VT[:, fc, 12:12 + KW].unsqueeze(2).to_broadcast([128, KW, 128]), op=ALU.mult),
                reads=["identb"] + VTK, writes=[K("DG", fc % 2)])
            bk = P.next_bank()
            mm_group(bk, [(dg[:, j, :], zA[:, fc, j:j + 512]) for j in range(KW)], [K("DG", fc % 2), K("zA", fc)])
            P.op("scalar", lambda e, fc=fc, bk=bk: e.activation(out=zc[:, fc, :], in_=psf(bk)[:], func=AF.Identity,
                                                                bias=VT[:, fc, 6:7], scale=1.0),
                 reads=[K("ps", bk)] + VTK, writes=[K("zc", fc)])
            P.op("scalar", lambda e, fc=fc, bk=bk: e.activation(out=sq[:, fc, :], in_=psf(bk)[:], func=AF.Square,
                                                                bias=VT[:, fc, 6:7], scale=1.0),
                 reads=[K("ps", bk)] + VTK, writes=[K("sq", fc)])
        zAk = [K("zA", fc) for fc in range(8)]
        P.op("gpsimd", lambda e: e.tensor_copy(out=zA[:, :, 0:30], in_=zA[:, :, 512:542]), reads=zAk, writes=zAk)
        if tb == 0:
            checkpoint("conv", [(zc[:].rearrange("p a b -> p (a b)"), 0), (sq[:].rearrange("p a b -> p (a b)"), 4096)])
        b1, b2 = P.next_bank(), P.next_bank()
        mm_group(b1, [(onesb[:], zc[:, fc, :]) for fc in range(8)], zck + ["onesb"])
        mm_group(b2, [(onesb[:], sq[:, fc, :]) for fc in range(8)], sqk + ["onesb"])
        mean_t, var_t = tf[0], tf[1]
        rs_t, nmr_t = var_t, mean_t
        P.op("vector", lambda e, b1=b1: e.tensor_scalar(out=mean_t[:], in0=psf(b1)[:], scalar1=1.0 / D, scalar2=None, op0=ALU.mult),
             reads=[K("ps", b1)], writes=["mean_t"])
        P.op("vector", lambda e: e.tensor_tensor(out=var_t[:], in0=mean_t[:], in1=mean_t[:], op=ALU.mult),
             reads=["mean_t"], writes=["var_t"])
        P.op("vector", lambda e, b2=b2: e.scalar_tensor_tensor(out=var_t[:], in0=psf(b2)[:], scalar=1.0 / D, in1=var_t[:],
                                                               op0=ALU.mult, op1=ALU.subtract),
             reads=[K("ps", b2), "var_t"], writes=["var_t"])
        P.op("scalar", lambda e: e.activation(out=var_t[:], in_=var_t[:], func=AF.Sqrt, bias=epsc[:, 0:1], scale=1.0),
             reads=["var_t", "epsc"], writes=["var_t"])
        P.op("vector", lambda e: e.reciprocal(out=rs_t[:], in_=var_t[:]), reads=["var_t"], writes=["var_t"])
        P.op("vector", lambda e: e.scalar_tensor_tensor(out=nmr_t[:], in0=mean_t[:], scalar=-1.0, in1=rs_t[:], op0=ALU.mult, op1=ALU.mult),
             reads=["mean_t", "var_t"], writes=["mean_t"])
        for fc in range(8):
            tn = tfn[fc % 2]
            tnk = K("tfn", fc % 2)
            P.op("vector", lambda e, fc=fc, tn=tn: e.tensor_tensor(out=tn[:], in0=zc[:, fc, :], in1=rs_t[:], op=ALU.mult),
                 reads=[K("zc", fc), "var_t"], writes=[tnk])
            P.op("gpsimd", lambda e, fc=fc, tn=tn: e.tensor_tensor(out=tn[:], in0=tn[:], in1=nmr_t[:], op=ALU.add),
                 reads=[tnk, "mean_t"], writes=[tnk])
            P.op("scalar", lambda e, fc=fc, tn=tn: e.activation(out=zs[:, fc, :], in_=tn[:], func=AF.Silu,
                                                               bias=VT[:, fc, 8:9], scale=VT[:, fc, 7:8]),
                 reads=[tnk] + VTK, writes=[K("zs", fc)])
        s = load_w(3)
        for a in range(4):
            for nh in range(2):
                bk = P.next_bank()
                mm_group(bk, [(hT[:, ko, a * 128:(a + 1) * 128], WS[s][:, ko, nh * 512:(nh + 1) * 512]) for ko in range(8)]
                         + [(onesb[0:33, :], bv_hl[0:33, nh * 512:(nh + 1) * 512])], wkeys(s) + hTk + ["bv_hl", "onesb"])
                P.op("scalar", lambda e, a=a, nh=nh, bk=bk: e.activation(out=vtok[:, a, nh * 512:(nh + 1) * 512], in_=psf(bk)[:],
                                                                       func=AF.Gelu),
                     reads=[K("ps", bk)], writes=[K("v", a, nh)])
        sk, rs, nbt = ln_stats4(lambda a: vtok[:, a, :], 1, lambda a: [K("v", a, 0), K("v", a, 1)], "lv")
        for a in range(4):
            vk = [K("v", a, 0), K("v", a, 1)]
            P.op("scalar", lambda e, a=a, rs=rs, nbt=nbt: e.activation(out=vtok[:, a, :], in_=vtok[:, a, :], func=AF.Identity,
                                                                       bias=nbt[:, a:a + 1], scale=rs[:, a:a + 1]),
                 reads=vk + sk, writes=vk)
            P.op("vector", lambda e, a=a: e.tensor_tensor(out=vtok[:, a, :], in0=vtok[:, a, :], in1=BC[:, BC_LVG, :], op=ALU.mult),
                 reads=vk + [K("BC", BC_LVG)], writes=vk)
            P.op("gpsimd", lambda e, a=a: e.tensor_tensor(out=vtok[:, a, :], in0=vtok[:, a, :], in1=BC[:, BC_LVB, :], op=ALU.add),
                 reads=vk + [K("BC", BC_LVB)], writes=vk)
        if tb == 0:
            checkpoint("v", [(vtok[:].rearrange("p a b -> p (a b)"), 0)])
        for gi, dst, name in ((4, sgA, "sgA"), (5, sgB, "sgB")):
            s = load_w(gi)
            for fc in range(8):
                bk = P.next_bank()
                mm_group(bk, [(WS[s][:, ko, fc * 128:(fc + 1) * 128], hT[:, ko, :]) for ko in range(8)], wkeys(s) + hTk)
                P.op("scalar", lambda e, fc=fc, bk=bk, dst=dst, gi=gi: e.activation(out=dst[:, fc, :], in_=psf(bk)[:], func=AF.Sigmoid,
                                                                                  bias=VT[:, fc, gi:gi + 1], scale=1.0),
                     reads=[K("ps", bk)] + VTK, writes=[K(name, fc)])
        if tb == 0:
            checkpoint("gates", [(sgA[:].rearrange("p a b -> p (a b)"), 0), (sgB[:].rearrange("p a b -> p (a b)"), 4096)])
        if tb == 0:
            checkpoint("lna", [(zs[:].rearrange("p a b -> p (a b)"), 0), (rs_t[:], 4096), (mean_t[:], 4608)])
        P.alias(gatedk, xnk)
        for a in range(4):
            vk = [K("v", a, 0), K("v", a, 1)]
            for hh in range(2):
                bk = P.next_bank()

                def mix(e, a=a, hh=hh, bk=bk):
                    ins = None
                    for q in range(4):
                        h = hh * 4 + q
                        e.matmul(psf(bk)[:, q * 128:(q + 1) * 128], lhsT=vtok[:, a, h * 128:(h + 1) * 128], rhs=WcT[:, h, :],
                                 start=True, stop=False)
                        ins = e.matmul(psf(bk)[:, q * 128:(q + 1) * 128], lhsT=onesb[0:33, :], rhs=bs_hl[0:33, h * 128:(h + 1) * 128],
                                       start=False, stop=True)
                    return ins
                P.op("tensor", mix, reads=vk + ["WcT", "bs_hl", "onesb"], writes=[K("ps", bk)])
                P.op("vector", lambda e, a=a, hh=hh, bk=bk: e.tensor_tensor(
                    out=gated[:, hh * 4:hh * 4 + 4, a * 128:(a + 1) * 128], in0=gu[:, hh * 4:hh * 4 + 4, a * 128:(a + 1) * 128],
                    in1=psf(bk)[:].rearrange("p (q t) -> p q t", q=4), op=ALU.mult),
                    reads=[K("ps", bk)] + [K("gu", fc) for fc in range(hh * 4, hh * 4 + 4)], writes=[K("gated", a, hh)])
        if tb == 0:
            checkpoint("mix", [(gated[:].rearrange("p a b -> p (a b)"), 0)])
        P.alias(tAk, zck)
        s = load_w(6)
        for fc in range(8):
            bk = P.next_bank()
            mm_group(bk, [(WS[s][:, ko, fc * 128:(fc + 1) * 128], zs[:, ko, :]) for ko in range(8)],
                     wkeys(s) + [K("zs", ko) for ko in range(8)])
            P.op("vector", lambda e, fc=fc, bk=bk: e.scalar_tensor_tensor(
                out=tA[:, fc, :], in0=psf(bk)[:], scalar=VT[:, fc, 9:10], in1=sgA[:, fc, :], op0=ALU.add, op1=ALU.mult),
                reads=[K("ps", bk), K("sgA", fc)] + VTK, writes=[K("tA", fc)])
        s = load_w(7)
        for fc in range(8):
            bk = P.next_bank()
            mm_group(bk, [(WS[s][:, ko, fc * 128:(fc + 1) * 128], gated[:, ko, :]) for ko in range(8)], wkeys(s) + gatedk)
            P.op("vector", lambda e, fc=fc, bk=bk: e.scalar_tensor_tensor(
                out=sgB[:, fc, :], in0=psf(bk)[:], scalar=VT[:, fc, 10:11], in1=sgB[:, fc, :], op0=ALU.add, op1=ALU.mult),
                reads=[K("ps", bk), K("sgB", fc)] + VTK, writes=[K("sgB", fc)])
            P.op("gpsimd", lambda e, fc=fc: e.tensor_tensor(out=merged[:, fc, :], in0=tA[:, fc, :], in1=sgB[:, fc, :], op=ALU.add),
                 reads=[K("tA", fc), K("sgB", fc)], writes=[K("tA", fc)])
        if tb == 0:
            checkpoint("merge", [(merged[:].rearrange("p a b -> p (a b)"), 0)])
        s = load_w(8)
        P.alias(xn2bk, sqk)
        for a in range(4):
            for nh in range(2):
                bk = P.next_bank()
                mm_group(bk, [(merged[:, ko, a * 128:(a + 1) * 128], WS[s][:, ko, nh * 512:(nh + 1) * 512]) for ko in range(8)]
                         + [(onesb[0:33, :], bo_hl[0:33, nh * 512:(nh + 1) * 512])],
                         wkeys(s) + tAk + ["bo_hl", "onesb"])
                P.op("vector", lambda e, a=a, nh=nh, bk=bk: e.scalar_tensor_tensor(
                    out=F1[:, a, nh * 512:(nh + 1) * 512], in0=F1[:, a, nh * 512:(nh + 1) * 512], scalar=ALPHA, in1=psf(bk)[:],
                    op0=ALU.mult, op1=ALU.add), reads=[K("ps", bk), K("F1", a)], writes=[K("F1", a)])
        sk, rs, nbt = ln_stats4(lambda a: F1[:, a, :], 2, lambda a: [K("F1", a)], "p1")
        for a in range(4):
            fk = [K("F1", a)]
            P.op("scalar", lambda e, a=a, rs=rs, nbt=nbt: e.activation(out=F1[:, a, :], in_=F1[:, a, :], func=AF.Identity,
                                                                       bias=nbt[:, a:a + 1], scale=rs[:, a:a + 1]),
                 reads=fk + sk, writes=fk)
            P.op("vector", lambda e, a=a: e.tensor_tensor(out=F1[:, a, :], in0=F1[:, a, :], in1=BC[:, BC_P1G, :], op=ALU.mult),
                 reads=fk + [K("BC", BC_P1G)], writes=fk)
            P.op("gpsimd", lambda e, a=a: e.tensor_tensor(out=F1[:, a, :], in0=F1[:, a, :], in1=BC[:, BC_P1B, :], op=ALU.add),
                 reads=fk + [K("BC", BC_P1B)], writes=fk)
            P.op("sync", lambda e, a=a, tb=tb: e.dma_start(out=x1_v[tb][:, a, :], in_=F1[:, a, :]), reads=fk,
                 writes=[K("x1s", tb, a)], dma_sem=f"x1s{a}")
        sk, rs, nbt = ln_stats4(lambda a: F1[:, a, :], 3, lambda a: [K("F1", a)], "l2")
        for a in range(4):
            fk = [K("F1", a)]
            xf = xn2f[a % 2]
            xfk = [K("xn2f", a % 2)]
            P.op("scalar", lambda e, a=a, rs=rs, nbt=nbt, xf=xf: e.activation(out=xf[:], in_=F1[:, a, :], func=AF.Identity,
                                                                              bias=nbt[:, a:a + 1], scale=rs[:, a:a + 1]),
                 reads=fk + sk, writes=xfk)
            P.op("gpsimd", lambda e, a=a, xf=xf: e.tensor_copy(out=xn2b[:, a, :], in_=xf[:]), reads=xfk, writes=[K("xn2b", a)])
            if tb + 1 < NB:
                head_tile(tb + 1, a)
            bks = [P.next_bank(), P.next_bank()]
            for half in range(2):
                bk = bks[half]

                def tr(e, half=half, bk=bk, xf=xf):
                    ins = None
                    for q in range(4):
                        ko = half * 4 + q
                        ins = e.transpose(out=psf(bk)[:, q * 128:(q + 1) * 128], in_=xf[:, ko * 128:(ko + 1) * 128], identity=identf[:])
                    return ins
                P.op("tensor", tr, reads=xfk + ["identf"], writes=[K("ps", bk)])
                hT2 = h2T[a % 2]
                for q in range(4):
                    ko = half * 4 + q
                    if half == 0:
                        P.op("vector", lambda e, ko=ko, q=q, bk=bk, hT2=hT2: e.tensor_scalar(
                            out=hT2[:, ko, :], in0=psf(bk)[:, q * 128:(q + 1) * 128], scalar1=VT[:, ko, C_SC2:C_SC2 + 1],
                            scalar2=VT[:, ko, C_SH2:C_SH2 + 1], op0=ALU.mult, op1=ALU.add),
                            reads=[K("ps", bk)] + VTK, writes=[K("h2T", ko)])
                    else:
                        P.op("scalar", lambda e, ko=ko, q=q, bk=bk, hT2=hT2: e.activation(
                            out=hT2[:, ko, :], in_=psf(bk)[:, q * 128:(q + 1) * 128], func=AF.Identity,
                            bias=VT[:, ko, C_SH2:C_SH2 + 1], scale=VT[:, ko, C_SC2:C_SC2 + 1]),
                            reads=[K("ps", bk)] + VTK, writes=[K("h2T", ko)])
            bk = P.next_bank()
            hT2 = h2T[a % 2]
            mm_group(bk, [(hT2[:, ko, :], wr[:, ko, :]) for ko in range(8)] + [(onesf[0:1, :], br_row[0:1, :])],
                     [K("h2T", ko) for ko in range(8)] + ["wr", "br_row", "onesf"], out_ap=psf(bk)[:, 0:E])
            j = tb * 4 + a
            lg = lgs[j % 2]
            P.op("vector", lambda e, bk=bk, lg=lg: e.tensor_copy(out=lg[:], in_=psf(bk)[:, 0:E]),
                 reads=[K("ps", bk)], writes=[K("lgs", j % 2)])
            P.op("sync", lambda e, j=j, lg=lg: e.dma_start(out=lg_scr[:, j * E:(j + 1) * E], in_=lg[:]), reads=[K("lgs", j % 2)],
                 writes=[K("lg_scr", j)], dma_sem=f"lgs{j % 2}")
        P.op("sync", lambda e, tb=tb: e.dma_start(out=xn2_v[tb], in_=xn2b[:]), reads=xn2bk, writes=[K("xn2s", tb)], dma_sem="xn2s")
        if tb == 0:
            checkpoint("blk0", [(F1[:].rearrange("p a b -> p (a b)"), 0)])

    if stage == 1:
        P.barrier()
        P.emit()
        return nc

    P.barrier()

    IOA = bass.IndirectOffsetOnAxis
    xtop = Arena(nc, X_off, LIMIT)
    posI, _ = xtop.alloc("posI", [128, 4, NT], I32)
    gw, _ = xtop.alloc("gw", [128, NT, 4], F32)
    Gd, _ = xtop.alloc("Gd", [128, NT, E], F32)
    idxU, _ = xtop.alloc("idxU", [128, NMB, 8], I32)
    bupT, _ = xtop.alloc("bupT", [128, 16, NMB], F32)
    bdn_sb, _ = xtop.alloc("bdn_sb", [32, D], F32)
    moe_base = xtop.ptr
    rt = Arena(nc, WT_off[0], X_off)
    L, _ = rt.alloc("L", [128, NT, E], F32)
    m8, _ = rt.alloc("m8", [128, NT, 8], F32)
    d4, _ = rt.alloc("d4", [128, NT, 4], F32)
    es, _ = rt.alloc("es", [128, NT], F32)
    Mb, _ = rt.alloc("Mb", [128, NT * E], BF16)
    Ust, _ = rt.alloc("Ust", [128, 128], BF16)
    ioU, _ = rt.alloc("ioU", [128, 128], I32)
    within, _ = rt.alloc("within", [128, NT, E], F32)
    cnt, _ = rt.alloc("cnt", [128, NT, E], F32)
    cum, _ = rt.alloc("cum", [128, NT, E], F32)
    pos, _ = rt.alloc("pos", [128, NT, E], F32)
    sel, _ = rt.alloc("sel", [128, NT, E], F32)
    prod, _ = rt.alloc("prod", [128, NT, E], F32)
    posk, _ = rt.alloc("posk", [128, 4, NT], F32)
    tot, _ = rt.alloc("tot", [128, E], F32)
    nblk, _ = rt.alloc("nblk", [128, E], F32)
    pend, _ = rt.alloc("pend", [128, E], F32)
    pstart, _ = rt.alloc("pstart", [128, E], F32)
    iob_i, _ = rt.alloc("iob_i", [128, NMB], I32)
    iob, _ = rt.alloc("iob", [128, NMB], F32)
    cmpb, _ = rt.alloc("cmpb", [128, NMB, E], F32)
    blke, _ = rt.alloc("blke", [128, NMB], F32)
    pk_i, _ = rt.alloc("pk_i", [128, 8], I32)
    pk_f, _ = rt.alloc("pk_f", [128, 8], F32)
    idxf, _ = rt.alloc("idxf", [128, NMB, 8], F32)
    pid_i, _ = rt.alloc("pid_i", [128, 1], I32)
    pid_f, _ = rt.alloc("pid_f", [128, 1], F32)
    OH, _ = rt.alloc("OH", [32, NMB], F32)
    bup_sb, _ = rt.alloc("bup_sb", [32, 2 * D], F32)

    def V_(fn, reads, writes):
        P.op("vector", fn, reads=reads, writes=writes)

    for slot, src in ((BC_P1G, post2_g), (BC_P1B, post2_b)):
        P.op("sync", lambda e, slot=slot, src=src: e.dma_start(out=BC[:, slot, :], in_=src.to_broadcast([128, D])),
             writes=[K("BC", slot)], dma_sem=f"BC{slot}")
    P.op("sync", lambda e: e.dma_start(out=L[:].rearrange("p j e -> p (j e)"), in_=lg_scr), writes=["L"], dma_sem="L")
    P.op("sync", lambda e: e.dma_start(out=bup_sb[:], in_=b_up), writes=["bup_sb"], dma_sem="bup_sb")
    P.op("sync", lambda e: e.dma_start(out=bdn_sb[:], in_=b_down), writes=["bdn_sb"], dma_sem="bdn_sb")
    V_(lambda e: e.tensor_tensor(out=bdn_sb[:], in0=bdn_sb[:], in1=BC[0:32, BC_G2, :], op=ALU.mult), ["bdn_sb"], ["bdn_sb"])
    P.op("gpsimd", lambda e: e.iota(ioU[:], pattern=[[1, 128]], base=0, channel_multiplier=-1), writes=["ioU"])
    P.op("gpsimd", lambda e: e.iota(iob_i[:], pattern=[[1, NMB]], base=0, channel_multiplier=0), writes=["iob_i"])
    P.op("gpsimd", lambda e: e.iota(pk_i[:], pattern=[[128, 8]], base=0, channel_multiplier=1), writes=["pk_i"])
    P.op("gpsimd", lambda e: e.iota(pid_i[:], pattern=[[0, 1]], base=0, channel_multiplier=1), writes=["pid_i"])
    V_(lambda e: e.tensor_scalar(out=Ust[:], in0=ioU[:], scalar1=0, scalar2=None, op0=ALU.is_gt), ["ioU"], ["Ust"])
    V_(lambda e: e.tensor_copy(out=iob[:], in_=iob_i[:]), ["iob_i"], ["iob"])
    V_(lambda e: e.tensor_copy(out=pk_f[:], in_=pk_i[:]), ["pk_i"], ["pk_f"])
    V_(lambda e: e.tensor_copy(out=pid_f[:], in_=pid_i[:]), ["pid_i"], ["pid_f"])
    for j in range(NT):
        V_(lambda e, j=j: e.max(out=m8[:, j, :], in_=L[:, j, :]), ["L"], [K("m8", j)])
    m8k = [K("m8", j) for j in range(NT)]
    V_(lambda e: e.tensor_tensor(out=d4[:], in0=m8[:, :, 0:4], in1=m8[:, :, 0:1].to_broadcast([128, NT, 4]), op=ALU.subtract), m8k, ["d4"])
    P.op("scalar", lambda e: e.activation(out=d4[:], in_=d4[:], func=AF.Exp), reads=["d4"], writes=["d4"])
    V_(lambda e: e.tensor_reduce(out=es[:], in_=d4[:], axis=AX.X, op=ALU.add), ["d4"], ["es"])
    V_(lambda e: e.reciprocal(out=es[:], in_=es[:]), ["es"], ["es"])
    V_(lambda e: e.tensor_tensor(out=gw[:], in0=d4[:], in1=es[:].unsqueeze(2).to_broadcast([128, NT, 4]), op=ALU.mult), ["d4", "es"], ["gw"])
    V_(lambda e: e.tensor_tensor(out=Mb[:].rearrange("p (j e) -> p j e", e=E), in0=L[:], in1=m8[:, :, 3:4].to_broadcast([128, NT, E]),
                                 op=ALU.is_ge), ["L"] + m8k, ["Mb"])
    for half in range(2):
        bw, bc = P.next_bank(), P.next_bank()
        mm_group(bw, [(Ust[:], Mb[:, half * 512:(half + 1) * 512])], ["Ust", "Mb"])
        mm_group(bc, [(onesb[:], Mb[:, half * 512:(half + 1) * 512])], ["onesb", "Mb"])
        V_(lambda e, half=half, bw=bw: e.tensor_copy(out=within[:, half * 16:(half + 1) * 16, :],
                                                    in_=psf(bw)[:].rearrange("p (j e) -> p j e", e=E)), [K("ps", bw)], [K("within", half)])
        V_(lambda e, half=half, bc=bc: e.tensor_copy(out=cnt[:, half * 16:(half + 1) * 16, :],
                                                    in_=psf(bc)[:].rearrange("p (j e) -> p j e", e=E)), [K("ps", bc)], [K("cnt", half)])
    wk = [K("within", 0), K("within", 1)]
    ck = [K("cnt", 0), K("cnt", 1)]
    P.op("gpsimd", lambda e: e.memset(cum[:, 0, :], 0.0), writes=["cum"])
    for j in range(1, NT):
        V_(lambda e, j=j: e.tensor_tensor(out=cum[:, j, :], in0=cum[:, j - 1, :], in1=cnt[:, j - 1, :], op=ALU.add), ["cum"] + ck, ["cum"])
    V_(lambda e: e.tensor_tensor(out=tot[:], in0=cum[:, NT - 1, :], in1=cnt[:, NT - 1, :], op=ALU.add), ["cum"] + ck, ["tot"])
    P.op("gpsimd", lambda e: e.memset(nblk[:], 0.0), writes=["nblk"])
    for m in range(8):
        V_(lambda e, m=m: e.scalar_tensor_tensor(out=nblk[:], in0=tot[:], scalar=float(MB * m), in1=nblk[:], op0=ALU.is_gt, op1=ALU.add),
           ["tot", "nblk"], ["nblk"])
    V_(lambda e: e.tensor_copy(out=pend[:, 0:1], in_=nblk[:, 0:1]), ["nblk"], ["pend"])
    for ee in range(1, E):
        V_(lambda e, ee=ee: e.tensor_tensor(out=pend[:, ee:ee + 1], in0=pend[:, ee - 1:ee], in1=nblk[:, ee:ee + 1], op=ALU.add),
           ["pend", "nblk"], ["pend"])
    V_(lambda e: e.tensor_tensor(out=pstart[:], in0=pend[:], in1=nblk[:], op=ALU.subtract), ["pend", "nblk"], ["pstart"])
    V_(lambda e: e.scalar_tensor_tensor(out=pos[:], in0=pstart[:].unsqueeze(1).to_broadcast([128, NT, E]), scalar=float(MB), in1=cum[:],
                                        op0=ALU.mult, op1=ALU.add), ["pstart", "cum"], ["pos"])
    V_(lambda e: e.tensor_tensor(out=pos[:], in0=pos[:], in1=within[:], op=ALU.add), ["pos"] + wk, ["pos"])
    for k in range(4):
        V_(lambda e, k=k: e.tensor_tensor(out=sel[:], in0=L[:], in1=m8[:, :, k:k + 1].to_broadcast([128, NT, E]), op=ALU.is_equal),
           ["L"] + m8k, ["sel"])
        V_(lambda e: e.tensor_tensor(out=prod[:], in0=sel[:], in1=pos[:], op=ALU.mult), ["sel", "pos"], ["prod"])
        V_(lambda e, k=k: e.tensor_reduce(out=posk[:, k, :], in_=prod[:], axis=AX.X, op=ALU.add), ["prod"], [K("posk", k)])
        if k == 0:
            V_(lambda e, k=k: e.tensor_tensor(out=Gd[:], in0=sel[:], in1=gw[:, :, k:k + 1].to_broadcast([128, NT, E]), op=ALU.mult),
               ["sel", "gw"], ["Gd"])
        else:
            V_(lambda e, k=k: e.tensor_tensor(out=prod[:], in0=sel[:], in1=gw[:, :, k:k + 1].to_broadcast([128, NT, E]), op=ALU.mult),
               ["sel", "gw", "prod"], ["prod"])
            V_(lambda e: e.tensor_tensor(out=Gd[:], in0=Gd[:], in1=prod[:], op=ALU.add), ["Gd", "prod"], ["Gd"])
    V_(lambda e: e.tensor_copy(out=posI[:], in_=posk[:]), [K("posk", k) for k in range(4)], ["posI"])
    V_(lambda e: e.tensor_tensor(out=cmpb[:], in0=pend[:].unsqueeze(1).to_broadcast([128, NMB, E]),
                                 in1=iob[:].unsqueeze(2).to_broadcast([128, NMB, E]), op=ALU.is_le), ["pend", "iob"], ["cmpb"])
    V_(lambda e: e.tensor_reduce(out=blke[:], in_=cmpb[:], axis=AX.X, op=ALU.add), ["cmpb"], ["blke"])
    V_(lambda e: e.tensor_scalar(out=blke[:], in0=blke[:], scalar1=float(E - 1), scalar2=None, op0=ALU.min), ["blke"], ["blke"])
    V_(lambda e: e.scalar_tensor_tensor(out=idxf[:], in0=blke[:].unsqueeze(2).to_broadcast([128, NMB, 8]), scalar=float(D),
                                        in1=pk_f[:].unsqueeze(1).to_broadcast([128, NMB, 8]), op0=ALU.mult, op1=ALU.add),
       ["blke", "pk_f"], ["idxf"])
    V_(lambda e: e.tensor_copy(out=idxU[:], in_=idxf[:]), ["idxf"], ["idxU"])
    V_(lambda e: e.tensor_scalar(out=OH[:], in0=blke[0:32, :], scalar1=pid_f[0:32, 0:1], scalar2=None, op0=ALU.is_equal),
       ["blke", "pid_f"], ["OH"])
    for half in range(2):
        bk = P.next_bank()

        def bm(e, half=half, bk=bk):
            ins = None
            for q in range(8):
                fc = half * 8 + q
                ins = e.matmul(psf(bk)[:, q * NMB:(q + 1) * NMB], lhsT=bup_sb[0:32, fc * 128:(fc + 1) * 128], rhs=OH[0:32, :],
                               start=True, stop=True)
            return ins
        P.op("tensor", bm, reads=["bup_sb", "OH"], writes=[K("ps", bk)])
        if half == 0:
            V_(lambda e, bk=bk: e.tensor_copy(out=bupT[:, 0:8, :], in_=psf(bk)[:].rearrange("p (q b) -> p q b", b=NMB)),
               [K("ps", bk)], [K("bupT", 0)])
        else:
            V_(lambda e, bk=bk: e.tensor_scalar(out=bupT[:, 8:16, :], in0=psf(bk)[:].rearrange("p (q b) -> p q b", b=NMB),
                                               scalar1=1.0, scalar2=None, op0=ALU.add), [K("ps", bk)], [K("bupT", 1)])
    bupk = [K("bupT", 0), K("bupT", 1)]

    if True:
        checkpoint("route", [(posk[:].rearrange("p a b -> p (a b)"), 0), (gw[:].rearrange("p a b -> p (a b)"), 128), (blke[:], 256),
                             (tot[:], 320), (pend[:], 352), (bupT[:, 0, :], 384), (bupT[:, 8, :], 448),
                             (Gd[:].rearrange("p a b -> p (a b)"), 1024)])

    xbs = [rt.alloc(f"xbs{i}", [128, D], BF16)[0] for i in range(4)]
    xn2_t = xn2_scr.rearrange("(j p) d -> j p d", p=128)
    for j in range(NT):
        s = j % 4
        P.op("sync", lambda e, j=j, s=s: e.dma_start(out=xbs[s][:], in_=xn2_t[j]), writes=[K("xbs", s)], dma_sem=f"xbs{s}")
        P.op("gpsimd", lambda e, j=j, s=s: [e.indirect_dma_start(
            out=xg_scr, out_offset=IOA(ap=posI[:, k, j:j + 1], axis=0), in_=xbs[s][:], in_offset=None) for k in range(4)],
            reads=[K("xbs", s), "posI"], writes=[K("xg_scr", j)], dma_sem=f"xgs{s}", n_inc=4)
    P.barrier()

    WU = [ar.at(WT_off[2 * i], f"WU{i}", [128, 8, 2 * D], BF16) for i in range(2)]
    WD = [ar.at(WT_off[4 + i], f"WD{i}", [128, 8, D], BF16) for i in range(2)]
    mo = Arena(nc, moe_base, LIMIT)
    xg = [mo.alloc(f"xg{i}", [128, 4, D], BF16)[0] for i in range(2)]
    xgT2 = [mo.alloc(f"xgT{i}", [128, 8, 512], BF16)[0] for i in range(2)]
    actT, _ = mo.alloc("actT", [128, 8, 512], BF16)
    g1t = [mo.alloc(f"g1t{i}", [128, 512], F32)[0] for i in range(2)]
    sgt2 = [mo.alloc(f"sgt2{i}", [128, 512], BF16)[0] for i in range(2)]
    l1t = [mo.alloc(f"l1t{i}", [128, 512], BF16)[0] for i in range(2)]
    yo = [mo.alloc(f"yo{i}", [128, D], F32)[0] for i in range(2)]
    xg_v = xg_scr.rearrange("(b a p) d -> b p a d", a=4, p=128)
    yg_v = yg_scr.rearrange("(b a p) d -> b a p d", a=4, p=128)

    def moe_loads(b):
        s = b % 2
        P.op("gpsimd", lambda e, b=b, s=s: [e.indirect_dma_start(
            out=WU[s][:, ko, :], out_offset=None, in_=w_up, in_offset=IOA(ap=idxU[:, b, ko:ko + 1], axis=0)) for ko in range(8)],
            reads=["idxU"], writes=[K("WU", s)], dma_sem=f"WU{s}", n_inc=8)
        P.op("gpsimd", lambda e, b=b, s=s: [e.indirect_dma_start(
            out=WD[s][:, ko, :], out_offset=None, in_=w_down, in_offset=IOA(ap=idxU[:, b, ko:ko + 1], axis=0)) for ko in range(8)],
            reads=["idxU"], writes=[K("WD", s)], dma_sem=f"WD{s}", n_inc=8)
        P.op("sync", lambda e, b=b, s=s: e.dma_start(out=xg[s][:], in_=xg_v[b]), writes=[K("xg", s)], dma_sem=f"xg{s}")

    def moe_transposes(b):
        s = b % 2
        xgT = xgT2[s]
        for kp in range(4):
            bk = P.next_bank()

            def tr(e, kp=kp, bk=bk, s=s):
                ins = None
                for q in range(2):
                    ko = kp * 2 + q
                    for a in range(4):
                        ins = e.transpose(out=psb(bk)[:, q * 512 + a * 128:q * 512 + (a + 1) * 128],
                                          in_=xg[s][:, a, ko * 128:(ko + 1) * 128], identity=identb[:])
                return ins
            P.op("tensor", tr, reads=[K("xg", s), "identb"], writes=[K("ps", bk)])
            for q in range(2):
                ko = kp * 2 + q
                if kp % 2 == 0:
                    P.op("vector", lambda e, ko=ko, q=q, bk=bk: e.tensor_scalar(
                        out=xgT[:, ko, :], in0=psb(bk)[:, q * 512:(q + 1) * 512], scalar1=VT[:, ko, C_SC2:C_SC2 + 1],
                        scalar2=VT[:, ko, C_SH2:C_SH2 + 1], op0=ALU.mult, op1=ALU.add),
                        reads=[K("ps", bk)], writes=[K("xgT", s, ko)])
                else:
                    P.op("scalar", lambda e, ko=ko, q=q, bk=bk: e.activation(
                        out=xgT[:, ko, :], in_=psb(bk)[:, q * 512:(q + 1) * 512], func=AF.Identity,
                        bias=VT[:, ko, C_SH2:C_SH2 + 1], scale=VT[:, ko, C_SC2:C_SC2 + 1]),
                        reads=[K("ps", bk)], writes=[K("xgT", s, ko)])

    n_moe = NMB if moe_blocks is None else moe_blocks
    moe_loads(0)
    moe_transposes(0)
    ycount = 0
    for b in range(n_moe):
        s = b % 2
        xgT = xgT2[s]
        if b + 1 < n_moe:
            moe_loads(b + 1)
        WUk = [K("WU", s)]
        WDk = [K("WD", s)]
        xgTk = [K("xgT", s, ko) for ko in range(8)]
        for fc in range(8):
            bG, bL = P.next_bank(), P.next_bank()
            mm_group(bG, [(WU[s][:, ko, fc * 128:(fc + 1) * 128], xgT[:, ko, :]) for ko in range(8)], WUk + xgTk)
            mm_group(bL, [(WU[s][:, ko, D + fc * 128:D + (fc + 1) * 128], xgT[:, ko, :]) for ko in range(8)], WUk + xgTk)
            t = fc % 2
            V_(lambda e, fc=fc, b=b, bG=bG, t=t: e.tensor_scalar(out=g1t[t][:], in0=psf(bG)[:], scalar1=bupT[:, fc, b:b + 1], scalar2=SW_LIMIT,
                                                               op0=ALU.add, op1=ALU.min), [K("ps", bG)] + bupk, [K("g1t", t)])
            P.op("scalar", lambda e, t=t: e.activation(out=sgt2[t][:], in_=g1t[t][:], func=AF.Sigmoid, scale=SW_ALPHA),
                 reads=[K("g1t", t)], writes=[K("sgt2", t)])
            V_(lambda e, fc=fc, b=b, bL=bL, t=t: e.tensor_scalar(out=l1t[t][:], in0=psf(bL)[:], scalar1=bupT[:, 8 + fc, b:b + 1],
                                                               scalar2=1.0 - SW_LIMIT, op0=ALU.add, op1=ALU.max),
               [K("ps", bL)] + bupk, [K("l1t", t)])
            V_(lambda e, t=t: e.tensor_tensor(out=g1t[t][:], in0=g1t[t][:], in1=sgt2[t][:], op=ALU.mult),
               [K("g1t", t), K("sgt2", t)], [K("g1t", t)])
            V_(lambda e, fc=fc, t=t: e.scalar_tensor_tensor(out=actT[:, fc, :], in0=l1t[t][:], scalar=1.0 + SW_LIMIT, in1=g1t[t][:],
                                                           op0=ALU.min, op1=ALU.mult), [K("l1t", t), K("g1t", t)], [K("actT", fc)])
        actk = [K("actT", fc) for fc in range(8)]
        if b + 1 < n_moe:
            moe_transposes(b + 1)
        for a in range(4):
            ys_ = ycount % 2
            ycount += 1
            for nh in range(2):
                bk = P.next_bank()
                mm_group(bk, [(actT[:, fc, a * 128:(a + 1) * 128], WD[s][:, fc, nh * 512:(nh + 1) * 512]) for fc in range(8)], WDk + actk)
                V_(lambda e, nh=nh, bk=bk, ys_=ys_: e.tensor_tensor(out=yo[ys_][:, nh * 512:(nh + 1) * 512], in0=psf(bk)[:],
                                                                   in1=BC[:, BC_G2, nh * 512:(nh + 1) * 512], op=ALU.mult),
                   [K("ps", bk)], [K("yo", ys_, nh)])
            P.op("sync", lambda e, b=b, a=a, ys_=ys_: e.dma_start(out=yg_v[b][a], in_=yo[ys_][:]),
                 reads=[K("yo", ys_, 0), K("yo", ys_, 1)], writes=[K("yg", b, a)], dma_sem=f"yo{ys_}")
    P.barrier()

    cb = Arena(nc, WT_off[0], X_off)
    yk = [cb.alloc(f"yk{i}", [128, 4, D], F32)[0] for i in range(4)]
    xr = [cb.alloc(f"xr{i}", [128, D], F32)[0] for i in range(2)]
    acc = [cb.alloc(f"acc{i}", [128, D], F32)[0] for i in range(2)]
    gT = [cb.alloc(f"gT{i}", [32, 128], F32)[0] for i in range(2)]
    x1_t = x1_scr.rearrange("(j p) d -> j p d", p=128)
    out_t = out_d.rearrange("(j p) d -> j p d", p=128)
    def comb_g(j):
        g = j % 4
        P.op("gpsimd", lambda e, j=j, g=g: [e.indirect_dma_start(
            out=yk[g][:, k, :], out_offset=None, in_=yg_scr, in_offset=IOA(ap=posI[:, k, j:j + 1], axis=0)) for k in range(4)],
            reads=[], writes=[K("yk", g)], dma_sem=f"yk{g}", n_inc=4)

    def comb_a(j):
        s = j % 2
        g = j % 4
        P.op("sync", lambda e, j=j, s=s: e.dma_start(out=xr[s][:], in_=x1_t[j]), writes=[K("xr", s)], dma_sem=f"xr{s}")
        bk = P.next_bank()
        P.op("tensor", lambda e, j=j, bk=bk: e.transpose(out=psf(bk)[0:32, 0:128], in_=Gd[:, j, :], identity=identf[:]),
             reads=["identf"], writes=[K("ps", bk)])
        V_(lambda e, s=s, bk=bk: e.tensor_copy(out=gT[s][:], in_=psf(bk)[0:32, 0:128]), [K("ps", bk)], [K("gT", s)])
        for nh in range(2):
            bk = P.next_bank()
            mm_group(bk, [(gT[s][:], bdn_sb[0:32, nh * 512:(nh + 1) * 512])], [K("gT", s)])
            V_(lambda e, j=j, s=s, nh=nh, bk=bk, g=g: e.scalar_tensor_tensor(
                out=acc[s][:, nh * 512:(nh + 1) * 512], in0=yk[g][:, 0, nh * 512:(nh + 1) * 512], scalar=gw[:, j, 0:1], in1=psf(bk)[:],
                op0=ALU.mult, op1=ALU.add), [K("ps", bk), K("yk", g)], [K("acc", s, nh)])
        ak = [K("acc", s, 0), K("acc", s, 1)]
        for k in range(1, 4):
            V_(lambda e, j=j, s=s, k=k, g=g: e.scalar_tensor_tensor(out=acc[s][:], in0=yk[g][:, k, :], scalar=gw[:, j, k:k + 1], in1=acc[s][:],
                                                                   op0=ALU.mult, op1=ALU.add), ak + [K("yk", g)], ak)
        V_(lambda e, s=s: e.scalar_tensor_tensor(out=acc[s][:], in0=xr[s][:], scalar=ALPHA, in1=acc[s][:], op0=ALU.mult, op1=ALU.add),
           ak + [K("xr", s)], ak)

    def comb_b(j):
        s = j % 2
        ak = [K("acc", s, 0), K("acc", s, 1)]
        a4 = j % 4
        sk, rs, nbt = ln_stats4(lambda a, s=s: acc[s][:], j % 2, lambda a: ak, f"fin{j % 2}_", tiles=(a4,), use_act=True)
        P.op("scalar", lambda e, s=s, a4=a4, rs=rs, nbt=nbt: e.activation(out=acc[s][:], in_=acc[s][:], func=AF.Identity,
                                                                         bias=nbt[:, a4:a4 + 1], scale=rs[:, a4:a4 + 1]),
             reads=ak + sk, writes=ak)
        V_(lambda e, s=s: e.tensor_tensor(out=acc[s][:], in0=acc[s][:], in1=BC[:, BC_P1G, :], op=ALU.mult), ak, ak)
        V_(lambda e, s=s: e.tensor_tensor(out=acc[s][:], in0=acc[s][:], in1=BC[:, BC_P1B, :], op=ALU.add), ak, ak)
        P.op("sync", lambda e, j=j, s=s: e.dma_start(out=out_t[j], in_=acc[s][:]), reads=ak, writes=[K("out", j)], dma_sem=f"out{s}")

    for j in range(3):
        comb_g(j)
    comb_a(0)
    for j in range(NT):
        if j + 3 < NT:
            comb_g(j + 3)
        if j + 1 < NT:
            comb_a(j + 1)
        comb_b(j)
    P.barrier()
    P.emit()
    return nc


def _in_maps(inputs, stage=3):
    f = lambda a: np.ascontiguousarray(np.asarray(a, dtype=np.float32))
    g = {k: f(v) for k, v in inputs.items()}
    shared = {
        "w_ada": g["w_ada"][0], "b_ada": g["b_ada"][0].reshape(1, -1),
        "w_in": g["w_in"][0], "b_in": g["b_in"][0].reshape(6, D),
        "conv_w": g["conv_w"][0], "conv_b": g["conv_b"][0].reshape(1, D),
        "ln_a_g": g["ln_a_g"][0].reshape(1, D), "ln_a_b": g["ln_a_b"][0].reshape(1, D),
        "w_pa": g["w_pa"][0], "b_pa": g["b_pa"][0].reshape(1, D),
        "ln_v_g": g["ln_v_g"][0].reshape(1, D), "ln_v_b": g["ln_v_b"][0].reshape(1, D),
        "w_s": g["w_s"][0], "b_s": g["b_s"][0].reshape(1, D),
        "w_pb": g["w_pb"][0], "b_pb": g["b_pb"][0].reshape(1, D),
        "w_out": g["w_out"][0], "b_out": g["b_out"][0].reshape(1, D),
        "post1_g": g["post1_g"][0].reshape(1, D), "post1_b": g["post1_b"][0].reshape(1, D),
        "w_router": g["w_router"][0], "b_router": g["b_router"][0].reshape(1, E),
        "w_up": g["w_up"][0].reshape(E * D, 2 * D), "b_up": g["b_up"][0],
        "w_down": g["w_down"][0].reshape(E * D, D), "b_down": g["b_down"][0],
        "post2_g": g["post2_g"][0].reshape(1, D), "post2_b": g["post2_b"][0].reshape(1, D),
    }
    if stage < 2:
        for k in ("w_up", "b_up", "w_down", "b_down", "post2_g", "post2_b"):
            shared.pop(k)
    maps = []
    for b in range(8):
        m = dict(shared)
        m["x"] = g["x"][b]
        m["c"] = g["c"][b].reshape(1, D)
        maps.append(m)
    return maps


def kernel(**inputs):
    nc = build_program(stage=3)
    res = run_bass_kernel_spmd(nc, _in_maps(inputs), core_ids=list(range(8)))
    return np.stack([np.asarray(r["out"], dtype=np.float32) for r in res.results], axis=0)
```
